# Optimizing a Trainium2 kernel written in Bass

```python
import jax
import jax.numpy as jnp
from jax import lax
import numpy as np


D_MODEL = 4096
BATCH = 8
SEQ = 2048
DEPTH = 2

N_MIXERS = 2
D_FF = 11008
FFN_RES_WEIGHT = 0.5
RMS_EPS = 1e-6
NEG_INF = -1e30
FORCED_SCORE = 1e30

NSA_HEADS = 32
NSA_GROUPS = 4
NSA_HPG = NSA_HEADS // NSA_GROUPS
NSA_HEAD_DIM = D_MODEL // NSA_HEADS
NSA_QD = NSA_HEADS * NSA_HEAD_DIM
NSA_KVD = NSA_GROUPS * NSA_HEAD_DIM
NSA_IN = NSA_QD + 6 * NSA_KVD + 3 * NSA_HEADS
CMP_BLOCK = 32
CMP_STRIDE = 16
SLC_BLOCK = 64
SLC_TOPN = 16
SLC_QCHUNK = 16
WINDOW = 512
WIN_QBLOCK = 128
ROPE_THETA = 10000.0

HGRN_HEADS = 32
HGRN_KEY_DIM = D_MODEL // HGRN_HEADS
HGRN_VAL_DIM = D_MODEL // HGRN_HEADS
HGRN_IN = 2 * HGRN_HEADS * HGRN_KEY_DIM + 2 * HGRN_HEADS * HGRN_VAL_DIM
HGRN_CHUNK = 64

N_NSA_LAYERS = (DEPTH + N_MIXERS - 1) // N_MIXERS
N_HGRN_LAYERS = DEPTH // N_MIXERS

kernel_name = 'nsa_hgrn2_macaron_hybrid'


def rms_norm(x, g):
    xf = x.astype(jnp.float32)
    y = xf * lax.rsqrt(jnp.mean(xf * xf, axis=-1, keepdims=True) + RMS_EPS)
    return (y * g.astype(jnp.float32)).astype(x.dtype)


def swiglu(x, w_gate, w_up, w_down):
    return (jax.nn.silu(x @ w_gate) * (x @ w_up)) @ w_down


def rope_tables(seq, dim):
    inv = ROPE_THETA ** (-jnp.arange(0, dim, 2, dtype=jnp.float32) / dim)
    ang = jnp.arange(seq, dtype=jnp.float32)[:, None] * inv[None, :]
    ang = jnp.concatenate([ang, ang], axis=-1)
    return jnp.cos(ang), jnp.sin(ang)


def apply_rope(x, cos, sin):
    xf = x.astype(jnp.float32)
    x1, x2 = jnp.split(xf, 2, axis=-1)
    rot = jnp.concatenate([-x2, x1], axis=-1)
    return (xf * cos + rot * sin).astype(x.dtype)


def nsa_mixer(h, w_in, q_norm, k_norm, cmp_pos, cmp_w1, cmp_w2, w_o, cos, sin):
    B, S, _ = h.shape
    dt = h.dtype
    f32 = jnp.float32
    G, HPG, DH = NSA_GROUPS, NSA_HPG, NSA_HEAD_DIM
    cuts = [NSA_QD + i * NSA_KVD for i in range(7)]
    q, kc, vc, ks, vs, kw, vw, gl = jnp.split(h @ w_in, cuts, axis=-1)

    q = q.reshape(B, S, NSA_HEADS, DH).transpose(0, 2, 1, 3)
    q = apply_rope(rms_norm(q, q_norm), cos, sin) * (DH ** -0.5)
    q = q.reshape(B, G, HPG, S, DH)

    def kv_heads(t):
        return t.reshape(B, S, G, DH).transpose(0, 2, 1, 3)

    kc = apply_rope(rms_norm(kv_heads(kc), k_norm[0]), cos, sin)
    ks = apply_rope(rms_norm(kv_heads(ks), k_norm[1]), cos, sin)
    kw = apply_rope(rms_norm(kv_heads(kw), k_norm[2]), cos, sin)
    vc, vs, vw = kv_heads(vc), kv_heads(vs), kv_heads(vw)
    pos = jnp.arange(S)

    n_cmp = (S - CMP_BLOCK) // CMP_STRIDE + 1
    cmp_start = np.arange(n_cmp) * CMP_STRIDE
    cmp_idx = cmp_start[:, None] + np.arange(CMP_BLOCK)[None, :]

    def compress(t, pe, w1, w2):
        blocks = t[:, :, cmp_idx] + pe
        flat = blocks.reshape(B, G, n_cmp, CMP_BLOCK * DH)
        return jax.nn.gelu(flat @ w1) @ w2

    k_cmp = compress(kc, cmp_pos[0], cmp_w1[0], cmp_w2[0])
    v_cmp = compress(vc, cmp_pos[1], cmp_w1[1], cmp_w2[1])
    s_cmp = jnp.einsum('bghtd,bgnd->bghtn', q, k_cmp, preferred_element_type=f32)
    cmp_valid = (cmp_start + CMP_BLOCK - 1)[None, :] <= pos[:, None]
    p_cmp = jax.nn.softmax(jnp.where(cmp_valid, s_cmp, NEG_INF), axis=-1) * cmp_valid
    o_cmp = jnp.einsum('bghtn,bgnd->bghtd', p_cmp.astype(dt), v_cmp)

    n_slc = S // SLC_BLOCK
    slc_start = np.arange(n_slc) * SLC_BLOCK
    overlap = ((cmp_start[:, None] < slc_start[None, :] + SLC_BLOCK)
               & (cmp_start[:, None] + CMP_BLOCK > slc_start[None, :])).astype(np.float32)
    imp = jnp.einsum('bghtn,nj->bgtj', p_cmp, jnp.asarray(overlap))
    blk = jnp.arange(n_slc)
    cur = pos // SLC_BLOCK
    forced = (blk[None, :] == 0) | (blk[None, :] == cur[:, None]) | (blk[None, :] == cur[:, None] - 1)
    causal_blk = (blk * SLC_BLOCK)[None, :] <= pos[:, None]
    imp = jnp.where(forced, FORCED_SCORE, jnp.where(causal_blk, imp, NEG_INF))
    top_n = min(SLC_TOPN, n_slc)
    _, sel = lax.top_k(imp, top_n)

    k_blk = ks.reshape(B, G, n_slc, SLC_BLOCK, DH)
    v_blk = vs.reshape(B, G, n_slc, SLC_BLOCK, DH)
    n_ch = S // SLC_QCHUNK
    q_ch = jnp.moveaxis(q.reshape(B, G, HPG, n_ch, SLC_QCHUNK, DH), 3, 0)
    sel_ch = jnp.moveaxis(sel.reshape(B, G, n_ch, SLC_QCHUNK, top_n), 2, 0)
    pos_ch = pos.reshape(n_ch, SLC_QCHUNK)
    gather = jax.vmap(jax.vmap(lambda blocks, ids: blocks[ids]))
    within = jnp.arange(SLC_BLOCK)

    def slc_step(args):
        qc, ic, tc = args
        kg = gather(k_blk, ic)
        vg = gather(v_blk, ic)
        sc = jnp.einsum('bghcd,bgcnsd->bghcns', qc, kg, preferred_element_type=f32)
        kpos = ic[..., None] * SLC_BLOCK + within
        ok = (kpos <= tc[:, None, None])[:, :, None]
        p = jax.nn.softmax(jnp.where(ok, sc, NEG_INF), axis=(-2, -1))
        return jnp.einsum('bghcns,bgcnsd->bghcd', p.astype(dt), vg)

    o_slc = lax.map(slc_step, (q_ch, sel_ch, pos_ch))
    o_slc = jnp.moveaxis(o_slc, 0, 3).reshape(B, G, HPG, S, DH)

    n_wb = S // WIN_QBLOCK
    span = WINDOW + WIN_QBLOCK
    kw_pad = jnp.pad(kw, ((0, 0), (0, 0), (WINDOW, 0), (0, 0)))
    vw_pad = jnp.pad(vw, ((0, 0), (0, 0), (WINDOW, 0), (0, 0)))
    q_wb = jnp.moveaxis(q.reshape(B, G, HPG, n_wb, WIN_QBLOCK, DH), 3, 0)
    qoff = jnp.arange(WIN_QBLOCK)
    koff = jnp.arange(span) - WINDOW

    def win_step(args):
        qb, n = args
        start = n * WIN_QBLOCK
        kb = lax.dynamic_slice_in_dim(kw_pad, start, span, axis=2)
        vb = lax.dynamic_slice_in_dim(vw_pad, start, span, axis=2)
        sc = jnp.einsum('bghqd,bgkd->bghqk', qb, kb, preferred_element_type=f32)
        qp = start + qoff
        kp = start + koff
        diff = qp[:, None] - kp[None, :]
        ok = (diff >= 0) & (diff < WINDOW) & (kp >= 0)[None, :]
        p = jax.nn.softmax(jnp.where(ok, sc, NEG_INF), axis=-1)
        return jnp.einsum('bghqk,bgkd->bghqd', p.astype(dt), vb)

    o_win = lax.map(win_step, (q_wb, jnp.arange(n_wb)))
    o_win = jnp.moveaxis(o_win, 0, 3).reshape(B, G, HPG, S, DH)

    gate = jax.nn.sigmoid(gl.astype(f32)).reshape(B, S, 3, G, HPG)
    gate = jnp.moveaxis(gate, 1, -1)[..., None]
    o = gate[:, 0] * o_cmp + gate[:, 1] * o_slc + gate[:, 2] * o_win
    o = o.astype(dt).reshape(B, NSA_HEADS, S, DH).transpose(0, 2, 1, 3).reshape(B, S, NSA_QD)
    return o @ w_o


def hgrn2_mixer(h, w_in, lb, o_norm, w_o):
    B, S, _ = h.shape
    dt = h.dtype
    f32 = jnp.float32
    H, DK, DV, C = HGRN_HEADS, HGRN_KEY_DIM, HGRN_VAL_DIM, HGRN_CHUNK
    N = S // C
    q, fz, i, gz = jnp.split(h @ w_in, [H * DK, 2 * H * DK, 2 * H * DK + H * DV], axis=-1)
    f = lb + (1.0 - lb) * jax.nn.sigmoid(fz.astype(f32))

    def chunked(t, d):
        return t.astype(f32).reshape(B, N, C, H, d).transpose(0, 3, 1, 2, 4)

    qc = chunked(q, DK)
    kc = chunked(1.0 - f, DK)
    logf = chunked(jnp.log(f), DK)
    vc = chunked(i, DV)
    g_cum = jnp.cumsum(logf, axis=3)
    g_last = g_cum[:, :, :, -1:, :]
    q_dec = qc * jnp.exp(g_cum)
    k_inv = kc * jnp.exp(-g_cum)
    k_tail = kc * jnp.exp(g_last - g_cum)
    causal = jnp.tril(jnp.ones((C, C), dtype=bool))
    a = jnp.where(causal, jnp.einsum('bhnid,bhnjd->bhnij', q_dec, k_inv), 0.0)
    o_intra = jnp.einsum('bhnij,bhnjv->bhniv', a, vc)
    ds = jnp.einsum('bhncd,bhncv->bhndv', k_tail, vc)
    decay = jnp.exp(g_last[:, :, :, 0, :])

    def step(state, inp):
        d, dsn = inp
        return state * d[..., None] + dsn, state

    s0 = jnp.zeros((B, H, DK, DV), f32)
    _, s_prev = lax.scan(step, s0, (jnp.moveaxis(decay, 2, 0), jnp.moveaxis(ds, 2, 0)))
    o_inter = jnp.einsum('bhnid,bhndv->bhniv', q_dec, jnp.moveaxis(s_prev, 0, 2))
    o = (o_intra + o_inter).transpose(0, 2, 3, 1, 4).reshape(B, S, H, DV)
    o = rms_norm(o, o_norm) * jax.nn.silu(gz.astype(f32).reshape(B, S, H, DV))
    return o.reshape(B, S, H * DV).astype(dt) @ w_o


def setup_inputs(seed: int = 0) -> dict:
    key = jax.random.key(seed)
    k = jax.random.split(key, 20)
    f32 = jnp.float32

    def nrm(kk, shape, scale):
        return jax.random.normal(kk, shape, f32) * scale

    def gain(kk, shape):
        return 1.0 + 0.02 * jax.random.normal(kk, shape, f32)

    return {
        'x': nrm(k[0], (BATCH, SEQ, D_MODEL), 1.0),
        'ffn_norm': gain(k[1], (DEPTH, 2, D_MODEL)),
        'ffn_w_gate': nrm(k[2], (DEPTH, 2, D_MODEL, D_FF), D_MODEL ** -0.5),
        'ffn_w_up': nrm(k[3], (DEPTH, 2, D_MODEL, D_FF), D_MODEL ** -0.5),
        'ffn_w_down': nrm(k[4], (DEPTH, 2, D_FF, D_MODEL), D_FF ** -0.5),
        'mix_norm': gain(k[5], (DEPTH, D_MODEL)),
        'nsa_w_in': nrm(k[6], (N_NSA_LAYERS, D_MODEL, NSA_IN), D_MODEL ** -0.5),
        'nsa_q_norm': gain(k[7], (N_NSA_LAYERS, NSA_HEAD_DIM)),
        'nsa_k_norm': gain(k[8], (N_NSA_LAYERS, 3, NSA_HEAD_DIM)),
        'nsa_cmp_pos': nrm(k[9], (N_NSA_LAYERS, 2, CMP_BLOCK, NSA_HEAD_DIM), 0.1),
        'nsa_cmp_w1': nrm(k[10], (N_NSA_LAYERS, 2, CMP_BLOCK * NSA_HEAD_DIM, NSA_HEAD_DIM), (CMP_BLOCK * NSA_HEAD_DIM) ** -0.5),
        'nsa_cmp_w2': nrm(k[11], (N_NSA_LAYERS, 2, NSA_HEAD_DIM, NSA_HEAD_DIM), NSA_HEAD_DIM ** -0.5),
        'nsa_w_o': nrm(k[12], (N_NSA_LAYERS, NSA_QD, D_MODEL), NSA_QD ** -0.5),
        'hgrn_w_in': nrm(k[13], (N_HGRN_LAYERS, D_MODEL, HGRN_IN), D_MODEL ** -0.5),
        'hgrn_lb_logits': nrm(k[14], (DEPTH, HGRN_HEADS * HGRN_KEY_DIM), 0.1),
        'hgrn_o_norm': gain(k[15], (N_HGRN_LAYERS, HGRN_VAL_DIM)),
        'hgrn_w_o': nrm(k[16], (N_HGRN_LAYERS, HGRN_HEADS * HGRN_VAL_DIM, D_MODEL), (HGRN_HEADS * HGRN_VAL_DIM) ** -0.5),
    }


def reference(x, ffn_norm, ffn_w_gate, ffn_w_up, ffn_w_down, mix_norm, nsa_w_in, nsa_q_norm, nsa_k_norm,
              nsa_cmp_pos, nsa_cmp_w1, nsa_cmp_w2, nsa_w_o, hgrn_w_in, hgrn_lb_logits, hgrn_o_norm, hgrn_w_o):
    cos, sin = rope_tables(x.shape[1], NSA_HEAD_DIM)
    p_lb = jax.nn.softmax(hgrn_lb_logits.astype(jnp.float32), axis=0)
    lower_bounds = jnp.cumsum(p_lb, axis=0) - p_lb[0:1]
    for layer in range(DEPTH):
        slot = layer // N_MIXERS
        x = x + FFN_RES_WEIGHT * swiglu(rms_norm(x, ffn_norm[layer, 0]), ffn_w_gate[layer, 0],
                                        ffn_w_up[layer, 0], ffn_w_down[layer, 0])
        h = rms_norm(x, mix_norm[layer])
        if layer % N_MIXERS == 0:
            x = x + nsa_mixer(h, nsa_w_in[slot], nsa_q_norm[slot], nsa_k_norm[slot], nsa_cmp_pos[slot],
                              nsa_cmp_w1[slot], nsa_cmp_w2[slot], nsa_w_o[slot], cos, sin)
        else:
            x = x + hgrn2_mixer(h, hgrn_w_in[slot], lower_bounds[layer], hgrn_o_norm[slot], hgrn_w_o[slot])
        x = x + FFN_RES_WEIGHT * swiglu(rms_norm(x, ffn_norm[layer, 1]), ffn_w_gate[layer, 1],
                                        ffn_w_up[layer, 1], ffn_w_down[layer, 1])
    return x
```

```python
from contextlib import ExitStack
from concourse.bass_utils import run_bass_kernel_spmd
import numpy as np
import concourse.bass as bass
import concourse.mybir as mybir

F32 = mybir.dt.float32
BF16 = mybir.dt.bfloat16
ALU = mybir.AluOpType
AF = mybir.ActivationFunctionType
AX = mybir.AxisListType

ENGS = ("pe", "act", "dve", "pool", "sp")


class Buf:
    __slots__ = ("name", "w", "r", "dsem", "dcnt", "excl")

    def __init__(self, name="", excl=False):
        self.name = name
        self.excl = excl
        self.w = {}
        self.r = {}
        self.dsem = None
        self.dcnt = 0


class Sched:
    def __init__(self, nc, stack):
        self.nc = nc
        self.stack = stack
        self.q = {e: [] for e in ENGS}
        self.cnt = {e: 0 for e in ENGS}
        self.sems = {}
        for e in ("pe", "act", "dve", "pool"):
            self.sems[e] = stack.enter_context(nc.semaphore("s_" + e))
        self.seen = {e: {} for e in ENGS}
        self.dma_sems = []
        self.free_dsems = []
        self.ndsem = 0

    def _collect(self, eng, reads, writes):
        waits = {}
        for b in reads:
            for k, v in b.w.items():
                if waits.get(k, 0) < v:
                    waits[k] = v
            if b.excl:
                for k, v in b.r.items():
                    if k != eng and waits.get(k, 0) < v:
                        waits[k] = v
        for b in writes:
            for d in (b.w, b.r):
                for k, v in d.items():
                    if waits.get(k, 0) < v:
                        waits[k] = v
        out = []
        seen = self.seen[eng]
        for k, v in waits.items():
            if eng == "pe" and k == "pe":
                continue
            if seen.get(k, 0) >= v:
                continue
            seen[k] = v
            out.append((k, v))
        return out

    def _semobj(self, k):
        return self.sems[k]

    def op(self, eng, fn, reads=(), writes=(), sig=True):
        waits = self._collect(eng, reads, writes)
        if sig:
            self.cnt[eng] += 1
            val = self.cnt[eng]
        else:
            val = self.cnt[eng] + 1
        for b in reads:
            if b.r.get(eng, 0) < val:
                b.r[eng] = val
        for b in writes:
            b.w = {eng: val}
            b.r = {}
        self.q[eng].append((waits, fn, (eng, 1) if sig else None))

    def dma(self, q, out_ap, in_ap, reads=(), writes=(), owner=None, **kw):
        if owner is None:
            owner = writes[0] if writes else reads[0]
        if owner.dsem is None:
            if self.free_dsems:
                key, c0 = self.free_dsems.pop()
            else:
                key, c0 = "d%d" % self.ndsem, 0
                self.ndsem += 1
                assert self.ndsem <= 90, "out of DMA semaphores"
                self.sems[key] = self.stack.enter_context(self.nc.semaphore(key))
            self.dma_sems.append(owner)
            owner.dsem = key
            owner.dcnt = c0
        waits = self._collect(q, reads, writes)
        owner.dcnt += 16
        key, val = owner.dsem, owner.dcnt
        for b in reads:
            if b.r.get(key, 0) < val:
                b.r[key] = val
        for b in writes:
            b.w = {key: val}
            b.r = {}

        def fn(e, out_ap=out_ap, in_ap=in_ap, kw=kw):
            return e.dma_start(out=out_ap, in_=in_ap, **kw)
        self.q[q].append((waits, fn, (key, 16)))

    def dma_multi_begin(self, buf):
        pass

    def barrier(self, dummy_ap):
        waits = []
        seen = self.seen["dve"]
        for e in ("pe", "act", "pool", "dve"):
            v = self.cnt[e]
            if v > 0 and seen.get(e, 0) < v:
                seen[e] = v
                waits.append((e, v))
        for b in self.dma_sems:
            if b.dcnt > 0 and seen.get(b.dsem, 0) < b.dcnt:
                seen[b.dsem] = b.dcnt
                waits.append((b.dsem, b.dcnt))
        self.cnt["dve"] += 1
        val = self.cnt["dve"]
        self.q["dve"].append((waits, lambda e: e.memset(dummy_ap, 0.0), ("dve", 1)))
        for e in ("pe", "act", "pool", "sp"):
            self.seen[e]["dve"] = val
            for k, v in seen.items():
                if self.seen[e].get(k, 0) < v:
                    self.seen[e][k] = v
            self.q[e].append(([("dve", val)], None, None))
        seen["dve"] = val
        for b in self.dma_sems:
            self.free_dsems.append((b.dsem, b.dcnt))
            b.dsem = None
        self.dma_sems = []

    def final_wait(self, eng="sp"):
        waits = []
        for b in self.dma_sems:
            if b.dcnt > 0:
                waits.append((b.dsem, b.dcnt))
        for k, c in self.free_dsems:
            if c > 0:
                waits.append((k, c))
        self.q[eng].append((waits, None, None))

    def emit(self):
        nc = self.nc
        sems = self.sems
        q = self.q

        def run(e, items):
            for waits, fn, inc in items:
                for k, v in waits:
                    e.wait_ge(sems[k], v)
                if fn is not None:
                    ins = fn(e)
                    if inc is not None:
                        ins.then_inc(sems[inc[0]], inc[1])

        with nc.Block() as block:
            @block.sync
            def _(e):
                run(e, q["sp"])

            @block.tensor
            def _(e):
                run(e, q["pe"])

            @block.scalar
            def _(e):
                run(e, q["act"])

            @block.vector
            def _(e):
                run(e, q["dve"])

            @block.gpsimd
            def _(e):
                run(e, q["pool"])


D = 4096
F = 11008
T = 2048
TT = 512
KC = D // 128
FC = F // 128
EPS = 1e-6


class Ctx:
    pass


def bview(arena, off, words, pattern=None, **kw):
    ap = arena[:, off:off + words].bitcast(BF16)
    if pattern:
        ap = ap.rearrange(pattern, **kw)
    return ap


def fview(arena, off, words, pattern=None, **kw):
    ap = arena[:, off:off + words]
    if pattern:
        ap = ap.rearrange(pattern, **kw)
    return ap


def setup_common(c):
    c.ident = bview(c.arena, 0, 64)
    c.dummy = c.arena[:, 64:65]
    c.b_ident = Buf("ident")
    c.S.dma("pool", c.ident, c.dr["ident"], writes=[c.b_ident])
    c.pbank = [Buf("bank%d" % i, excl=True) for i in range(8)]
    c.BASE = 128


def psum_f(c, bank, n=512, off=0):
    return c.psum[:, bank * 512 + off: bank * 512 + off + n]


def psum_b(c, bank):
    return c.psum[:, bank * 512:(bank + 1) * 512].bitcast(BF16)


def rms_to_hT(c, x_ap, gain_row_ap, hT, b_hT, base, t0, ntok=TT):
    S = c.S
    A = c.arena
    xt = [fview(A, base + i * 4096, 4096) for i in range(2)]
    hb = [bview(A, base + 8192 + i * 2048, 2048) for i in range(2)]
    gb = fview(A, base + 12288, 4096)
    ss = fview(A, base + 16384, 8)
    b_xt = [Buf("xt0"), Buf("xt1")]
    b_hb = [Buf("hb0"), Buf("hb1")]
    b_gb = Buf("gb")
    b_ss = Buf("ss")
    S.dma("sp", gb, gain_row_ap.partition_broadcast(128), writes=[b_gb])
    nts = ntok // 128
    for ts in range(nts):
        i = ts % 2
        S.dma("sp", xt[i], x_ap[t0 + ts * 128: t0 + (ts + 1) * 128, :], writes=[b_xt[i]])
        S.op("dve", lambda e: e.memset(ss[:, 0:1], 0.0), writes=[b_ss])
        S.op("act", lambda e, i=i: e.activation(out=hb[i], in_=xt[i], func=AF.Square, accum_out=ss[:, 0:1]),
             reads=[b_xt[i]], writes=[b_hb[i], b_ss])
        S.op("dve", lambda e: e.tensor_scalar(out=ss[:, 1:2], in0=ss[:, 0:1], scalar1=1.0 / D, scalar2=EPS,
                                              op0=ALU.mult, op1=ALU.add), reads=[b_ss], writes=[b_ss])
        S.op("act", lambda e: e.activation(out=ss[:, 3:4], in_=ss[:, 1:2], func=AF.Sqrt), reads=[b_ss], writes=[b_ss])
        S.op("dve", lambda e: e.reciprocal(out=ss[:, 2:3], in_=ss[:, 3:4]), reads=[b_ss], writes=[b_ss])
        S.op("dve", lambda e, i=i: e.scalar_tensor_tensor(out=hb[i], in0=xt[i], scalar=ss[:, 2:3], in1=gb,
                                                          op0=ALU.mult, op1=ALU.mult),
             reads=[b_xt[i], b_gb, b_ss], writes=[b_hb[i]])
        for q4 in range(4):
            bank = q4
            pb = psum_b(c, bank).rearrange("p (k n) -> p k n", k=8)
            for j in range(8):
                kc = q4 * 8 + j
                S.op("pe", lambda e, i=i, kc=kc, j=j, pb=pb: e.transpose(out=pb[:, j, :], in_=hb[i][:, kc * 128:(kc + 1) * 128],
                                                                      identity=c.ident),
                     reads=[b_hb[i], c.b_ident], writes=[c.pbank[bank]], sig=(j == 7))
            eng = "act" if q4 % 2 == 0 else "dve"
            dst = hT[:, q4 * 8:(q4 + 1) * 8, ts * 128:(ts + 1) * 128]
            if eng == "act":
                S.op("act", lambda e, dst=dst, pb=pb: e.copy(out=dst, in_=pb), reads=[c.pbank[bank]], writes=[b_hT])
            else:
                S.op("dve", lambda e, dst=dst, pb=pb: e.tensor_copy(out=dst, in_=pb), reads=[c.pbank[bank]], writes=[b_hT])


def ffn_phase(c, x_in, x_out, gain_row, wg, wu, wd, ntiles=T // TT):
    S = c.S
    A = c.arena
    B0 = c.BASE
    hT = bview(A, B0, 8192, "p (k n) -> p k n", k=KC)
    o_aT = B0 + 8192
    aT = bview(A, o_aT, 22016, "p (k n) -> p k n", k=FC)
    o_ring = o_aT + 22016
    ring = [bview(A, o_ring + i * 4096, 4096, "p (k n) -> p k n", k=32) for i in range(4)]
    o_sl = o_ring + 16384
    sil = [fview(A, o_sl + i * 512, 512) for i in range(2)]
    o_xi = o_sl + 1024
    xin = [fview(A, o_xi + i * 1024, 1024, "p (t n) -> p t n", t=4) for i in range(2)]
    o_xo = o_xi + 2048
    xout = [fview(A, o_xo + i * 1024, 1024, "p (t n) -> p t n", t=4) for i in range(2)]
    assert o_xo + 2048 <= 53000

    wg_v = wg.rearrange("(k p) n -> p k n", p=128)
    wu_v = wu.rearrange("(k p) n -> p k n", p=128)
    wd_v = wd.rearrange("(k p) n -> p k n", p=128)
    kparts = [(0, 32), (32, 32), (64, 22)]

    for tt in range(ntiles):
        t0 = tt * TT
        b_hT = Buf("hT")
        b_aT = Buf("aT")
        b_ring = [Buf("ring%d" % i) for i in range(4)]
        b_sil = [Buf("sil0"), Buf("sil1")]
        b_xin = [Buf("xin0"), Buf("xin1")]
        b_xout = [Buf("xout0"), Buf("xout1")]
        rms_to_hT(c, x_in, gain_row, hT, b_hT, o_aT, t0)
        S.barrier(c.dummy)
        if getattr(c, "dbg", None) and tt == 0:
            S.dma("pool", c.dbg["hT"], A[:, B0:B0 + 8192].bitcast(BF16), reads=[b_hT], owner=Buf("dbg1"))
        ri = 0
        for fb in range(F // 256):
            sg, su = ri % 4, (ri + 1) % 4
            ri += 2
            S.dma("pool", ring[sg], wg_v[:, :, fb * 256:(fb + 1) * 256], writes=[b_ring[sg]])
            S.dma("pool", ring[su], wu_v[:, :, fb * 256:(fb + 1) * 256], writes=[b_ring[su]])
            pb0 = (fb % 2) * 4
            for cc in range(2):
                for which, slot in ((0, sg), (1, su)):
                    bank = pb0 + which * 2 + cc
                    po = psum_f(c, bank)
                    for kc in range(KC):
                        S.op("pe", lambda e, po=po, slot=slot, kc=kc, cc=cc: e.matmul(
                            po, lhsT=ring[slot][:, kc, cc * 128:(cc + 1) * 128], rhs=hT[:, kc, :],
                            start=(kc == 0), stop=(kc == KC - 1)),
                            reads=[b_ring[slot], b_hT], writes=[c.pbank[bank]], sig=(kc == KC - 1))
            for cc in range(2):
                fc = fb * 2 + cc
                si = fc % 2
                pg = psum_f(c, pb0 + cc)
                pu = psum_f(c, pb0 + 2 + cc)
                S.op("act", lambda e, si=si, pg=pg: e.activation(out=sil[si], in_=pg, func=AF.Silu),
                     reads=[c.pbank[pb0 + cc]], writes=[b_sil[si]])
                S.op("dve", lambda e, si=si, pu=pu, fc=fc: e.tensor_tensor(out=aT[:, fc, :], in0=sil[si], in1=pu, op=ALU.mult),
                     reads=[b_sil[si], c.pbank[pb0 + 2 + cc]], writes=[b_aT])
        if getattr(c, "dbg", None) and tt == 0:
            S.dma("pool", c.dbg["aT"], A[:, o_aT:o_aT + 22016].bitcast(BF16), reads=[b_aT], owner=Buf("dbg2"))
        for ng in range(D // 256):
            xi = ng % 2
            src = x_in[t0:t0 + TT, ng * 256:(ng + 1) * 256].rearrange("(t p) n -> p t n", p=128)
            S.dma("sp", xin[xi], src, writes=[b_xin[xi]])
            pb0 = (ng % 2) * 4
            for (k0, kn) in kparts:
                s = ri % 4
                ri += 1
                S.dma("pool", ring[s][:, 0:kn, :], wd_v[:, k0:k0 + kn, ng * 256:(ng + 1) * 256], writes=[b_ring[s]])
                for ts in range(4):
                    bank = pb0 + ts
                    po = psum_f(c, bank, 256, 0)
                    for kl in range(kn):
                        kc = k0 + kl
                        S.op("pe", lambda e, po=po, s=s, kl=kl, kc=kc, ts=ts: e.matmul(
                            po, lhsT=aT[:, kc, ts * 128:(ts + 1) * 128], rhs=ring[s][:, kl, :],
                            start=(kc == 0), stop=(kc == FC - 1)),
                            reads=[b_ring[s], b_aT], writes=[c.pbank[bank]], sig=(kl == kn - 1))
            for ts in range(4):
                bank = pb0 + ts
                po = psum_f(c, bank, 256, 0)
                S.op("dve", lambda e, po=po, xi=xi, ts=ts: e.scalar_tensor_tensor(
                    out=xout[xi][:, ts, :], in0=po, scalar=0.5, in1=xin[xi][:, ts, :], op0=ALU.mult, op1=ALU.add),
                    reads=[c.pbank[bank], b_xin[xi]], writes=[b_xout[xi]])
            dst = x_out[t0:t0 + TT, ng * 256:(ng + 1) * 256].rearrange("(t p) n -> p t n", p=128)
            S.dma("sp", dst, xout[xi], reads=[b_xout[xi]])
        S.barrier(c.dummy)


class Pipe:
    def __init__(self):
        self.items = []

    def add(self, stages):
        self.items.append(stages)

    def run(self, interleave=False):
        n = len(self.items)
        if n == 0:
            return
        ns = max(len(s) for s in self.items)
        for step in range(n + ns - 1):
            lists = []
            for k in range(ns - 1, -1, -1):
                b = step - k
                if 0 <= b < n and k < len(self.items[b]) and self.items[b][k] is not None:
                    r = self.items[b][k]()
                    if interleave and r:
                        lists.append(r)
            if interleave and lists:
                keyed = []
                for li, L in enumerate(lists):
                    m = len(L)
                    for j, th in enumerate(L):
                        keyed.append(((j + 0.5) / m, li, j, th))
                keyed.sort(key=lambda t: (t[0], t[1], t[2]))
                for _, _, _, th in keyed:
                    th()
        self.items = []


class Rot:
    def __init__(self, views):
        self.v = views
        self.b = [Buf("rot") for _ in views]

    def get(self, i):
        k = i % len(self.v)
        return self.v[k], self.b[k]


class Ring:
    def __init__(self, c, off):
        self.tiles = [bview(c.arena, off + i * 4096, 4096, "p (k n) -> p k n", k=32) for i in range(4)]
        self.bufs = [Buf("ring%d" % i) for i in range(4)]
        self.i = 0

    def load(self, c, src_ap, kn, ncol):
        s = self.i % 4
        self.i += 1
        c.S.dma("pool", self.tiles[s][:, 0:kn, 0:ncol], src_ap, writes=[self.bufs[s]])
        return self.tiles[s], self.bufs[s]


def mm_fm(c, ring, hT, b_hT, w_v, col0, nchunks, banks, ntok=TT, nk=KC):
    wt, wb = ring.load(c, w_v[:, 0:nk, col0:col0 + nchunks * 128], nk, nchunks * 128)
    for cc in range(nchunks):
        po = psum_f(c, banks[cc], ntok)
        for kc in range(nk):
            c.S.op("pe", lambda e, po=po, wt=wt, kc=kc, cc=cc: e.matmul(
                po, lhsT=wt[:, kc, cc * 128:(cc + 1) * 128], rhs=hT[:, kc, 0:ntok], start=(kc == 0), stop=(kc == nk - 1)),
                reads=[wb, b_hT], writes=[c.pbank[banks[cc]]], sig=(kc == nk - 1))


def mm_tm(c, ring, aT, b_aT, nk, w_v, col0, ncol, banks, nts=4, kparts=None):
    if kparts is None:
        kparts = [(k0, min(32, nk - k0)) for k0 in range(0, nk, 32)]
    for (k0, kn) in kparts:
        wt, wb = ring.load(c, w_v[:, k0:k0 + kn, col0:col0 + ncol], kn, ncol)
        for ts in range(nts):
            po = psum_f(c, banks[ts], ncol)
            for kl in range(kn):
                kc = k0 + kl
                c.S.op("pe", lambda e, po=po, wt=wt, kl=kl, kc=kc, ts=ts: e.matmul(
                    po, lhsT=aT[:, kc, ts * 128:(ts + 1) * 128], rhs=wt[:, kl, 0:ncol],
                    start=(kc == 0), stop=(kc == nk - 1)),
                    reads=[wb, b_aT], writes=[c.pbank[banks[ts]]], sig=(kl == kn - 1))


def out_proj_phase(c, oT_dram, w_o, x_in, x_out, scale, ntiles=T // TT):
    S = c.S
    A = c.arena
    B0 = c.BASE
    oT = bview(A, B0, 8192, "p (k n) -> p k n", k=KC)
    ring = Ring(c, B0 + 8192)
    o_xi = B0 + 8192 + 16384
    xin = [fview(A, o_xi + i * 1024, 1024, "p (t n) -> p t n", t=4) for i in range(2)]
    xout = [fview(A, o_xi + 2048 + i * 1024, 1024, "p (t n) -> p t n", t=4) for i in range(2)]
    b_xin = [Buf("xin0"), Buf("xin1")]
    b_xout = [Buf("xout0"), Buf("xout1")]
    w_v = w_o.rearrange("(k p) n -> p k n", p=128)
    oT_v = oT_dram.rearrange("(k p) t -> p k t", p=128)
    for tt in range(ntiles):
        t0 = tt * TT
        b_oT = Buf("oT")
        S.dma("sp", oT, oT_v[:, :, t0:t0 + TT], writes=[b_oT])
        for ng in range(D // 256):
            xi = ng % 2
            src = x_in[t0:t0 + TT, ng * 256:(ng + 1) * 256].rearrange("(t p) n -> p t n", p=128)
            S.dma("sp", xin[xi], src, writes=[b_xin[xi]])
            banks = [(ng % 2) * 4 + ts for ts in range(4)]
            mm_tm(c, ring, oT, b_oT, KC, w_v, ng * 256, 256, banks)
            for ts in range(4):
                po = psum_f(c, banks[ts], 256)
                S.op("dve", lambda e, po=po, xi=xi, ts=ts: e.scalar_tensor_tensor(
                    out=xout[xi][:, ts, :], in0=po, scalar=scale, in1=xin[xi][:, ts, :], op0=ALU.mult, op1=ALU.add),
                    reads=[c.pbank[banks[ts]], b_xin[xi]], writes=[b_xout[xi]])
            dst = x_out[t0:t0 + TT, ng * 256:(ng + 1) * 256].rearrange("(t p) n -> p t n", p=128)
            S.dma("sp", dst, xout[xi], reads=[b_xout[xi]])
    S.barrier(c.dummy)


NSA_IN = 7264
QSCALE = 128 ** -0.5


def nsa_proj_phase(c, x_in, gain_row, w_in, q_norm, k_norm, scr, ntiles=T // TT):
    S = c.S
    A = c.arena
    B0 = c.BASE
    o = B0
    hT = bview(A, o, 8192, "p (k n) -> p k n", k=KC); o += 8192
    o_rms = o; o += 16400
    ring = Ring(c, o); o += 16384
    cosT = fview(A, o, 512); o += 512
    sinT = fview(A, o, 512); o += 512
    cosg = [fview(A, o + i * 512, 512) for i in range(4)]; o += 2048
    Pg = [bview(A, o + i * 64, 64) for i in range(4)]; o += 256
    ones_b = bview(A, o, 64); o += 64
    prot_f = fview(A, o, 128); o += 128
    gcol = fview(A, o, 4); o += 4
    epsc = fview(A, o, 1); o += 1
    vb = [bview(A, o + i * 512, 512, "p (t n) -> p t n", t=4) for i in range(2)]; o += 1024
    b_vb = [Buf("vb0"), Buf("vb1")]
    gt = fview(A, o, 384, "p (t n) -> p t n", t=4); o += 384
    b_gt = Buf("gt")
    assert o <= 53000, o
    NST = 7
    oo = o_rms
    R = {}
    for nm, words, bf in (("sq", 256, True), ("xb", 256, True), ("t1", 512, False), ("t2", 512, False), ("rs", 512, False), ("ob", 256, True)):
        views = []
        for i in range(NST):
            views.append(bview(A, oo, words) if bf else fview(A, oo, words))
            oo += words
        R[nm] = Rot(views)
    assert oo <= o_rms + 16400, oo

    b_const = Buf("nsa_const")
    S.dma("sp", prot_f, c.dr["prot"], writes=[b_const])
    S.op("dve", lambda e: e.memset(ones_b, 1.0 / 128), writes=[b_const])
    S.op("dve", lambda e: e.memset(epsc, EPS), writes=[b_const])
    S.dma("sp", gcol[:, 0:1], q_norm.rearrange("o d -> d o"), writes=[b_const])
    for i in range(3):
        S.dma("sp", gcol[:, 1 + i:2 + i], k_norm[0, i:i + 1, :].rearrange("o d -> d o"), writes=[b_const])
    S.op("dve", lambda e: e.tensor_scalar(out=gcol[:, 0:1], in0=gcol[:, 0:1], scalar1=QSCALE, scalar2=None, op0=ALU.mult),
         reads=[b_const], writes=[b_const])
    for i in range(4):
        S.op("dve", lambda e, i=i: e.tensor_scalar(out=Pg[i], in0=prot_f, scalar1=gcol[:, i:i + 1], scalar2=None, op0=ALU.mult),
             reads=[b_const], writes=[b_const])

    w_v = w_in.rearrange("(k p) n -> p k n", p=128)
    blocks = []
    for hp in range(16):
        blocks.append((hp * 256, [("rope", 0, scr["qT"][2 * hp + j]) for j in range(2)]))
    for gp in range(2):
        blocks.append((4096 + gp * 256, [("rope", 1, scr["kT"][0, 2 * gp + j]) for j in range(2)]))
        blocks.append((4608 + gp * 256, [("copy", 0, scr["vcT"][2 * gp + j]) for j in range(2)]))
        blocks.append((5120 + gp * 256, [("rope", 2, scr["kT"][1, 2 * gp + j]) for j in range(2)]))
        blocks.append((6144 + gp * 256, [("rope", 3, scr["kT"][2, 2 * gp + j]) for j in range(2)]))

    for tt in range(ntiles):
        t0 = tt * TT
        b_hT = Buf("hT")
        b_rope = Buf("rope_tiles")
        rms_to_hT(c, x_in, gain_row, hT, b_hT, o_rms, t0)
        S.barrier(c.dummy)
        S.dma("sp", cosT, c.dr["cosT"][:, t0:t0 + TT], writes=[b_rope])
        S.dma("sp", sinT, c.dr["sinT"][:, t0:t0 + TT], writes=[b_rope])
        b_cosg = Buf("cosg")
        for i in range(4):
            S.op("dve", lambda e, i=i: e.tensor_scalar(out=cosg[i], in0=cosT, scalar1=gcol[:, i:i + 1], scalar2=None, op0=ALU.mult),
                 reads=[b_rope, b_const], writes=[b_cosg])
        pipe = Pipe()
        item = 0
        wstate = {}
        for bi, (col0, subs) in enumerate(blocks):
            for cc, (kind, gi, dst) in enumerate(subs):
                it = item
                item += 1
                bx = it % 2
                px = psum_f(c, bx)
                bs = 2 + (it % 2) * 2
                br = bs + 1
                ps_, pr_ = psum_f(c, bs), psum_f(c, br)
                dst_ap = dst[:, t0:t0 + TT]
                sq, b_sq = R["sq"].get(it)
                xb, b_xb = R["xb"].get(it)
                t1, b_t1 = R["t1"].get(it)
                t2, b_t2 = R["t2"].get(it)
                rs, b_rs = R["rs"].get(it)
                ob, b_ob = R["ob"].get(it)

                def s0(bi=bi, cc=cc, col0=col0, px=px, bx=bx):
                    if cc == 0:
                        wstate[bi] = ring.load(c, w_v[:, 0:KC, col0:col0 + 256], KC, 256)
                    wt, wb = wstate[bi]
                    for kc in range(KC):
                        S.op("pe", lambda e, kc=kc: e.matmul(px, lhsT=wt[:, kc, cc * 128:(cc + 1) * 128], rhs=hT[:, kc, :],
                                                             start=(kc == 0), stop=(kc == KC - 1)),
                             reads=[wb, b_hT], writes=[c.pbank[bx]], sig=(kc == KC - 1))

                if kind == "copy":
                    def s1(px=px, bx=bx, ob=ob, b_ob=b_ob):
                        S.op("act", lambda e: e.copy(out=ob, in_=px), reads=[c.pbank[bx]], writes=[b_ob])

                    def s2(dst_ap=dst_ap, ob=ob, b_ob=b_ob):
                        S.dma("sp", dst_ap, ob, reads=[b_ob])
                    pipe.add([s0, s1, s2])
                    continue

                def s1(px=px, bx=bx, sq=sq, b_sq=b_sq, xb=xb, b_xb=b_xb, t1=t1, b_t1=b_t1, gi=gi):
                    S.op("act", lambda e: e.activation(out=sq, in_=px, func=AF.Square), reads=[c.pbank[bx]], writes=[b_sq])
                    S.op("act", lambda e: e.copy(out=xb, in_=px), reads=[c.pbank[bx]], writes=[b_xb])
                    S.op("dve", lambda e: e.tensor_tensor(out=t1, in0=px, in1=cosg[gi], op=ALU.mult),
                         reads=[c.pbank[bx], b_cosg], writes=[b_t1])

                def s2(ps_=ps_, pr_=pr_, bs=bs, br=br, sq=sq, b_sq=b_sq, xb=xb, b_xb=b_xb, gi=gi):
                    S.op("pe", lambda e: e.matmul(ps_, lhsT=ones_b, rhs=sq, start=True, stop=True),
                         reads=[b_sq, b_const], writes=[c.pbank[bs]])
                    S.op("pe", lambda e: e.matmul(pr_, lhsT=Pg[gi], rhs=xb, start=True, stop=True),
                         reads=[b_xb, b_const], writes=[c.pbank[br]])

                def s3(ps_=ps_, pr_=pr_, bs=bs, br=br, rs=rs, b_rs=b_rs, t2=t2, b_t2=b_t2):
                    S.op("act", lambda e: e.activation(out=rs, in_=ps_, func=AF.Sqrt, bias=epsc[:, 0:1]), reads=[c.pbank[bs], b_const], writes=[b_rs])
                    S.op("dve", lambda e: e.tensor_tensor(out=t2, in0=pr_, in1=sinT, op=ALU.mult),
                         reads=[c.pbank[br], b_rope], writes=[b_t2])

                def s4(rs=rs, b_rs=b_rs, t1=t1, b_t1=b_t1, t2=t2, b_t2=b_t2):
                    S.op("dve", lambda e: e.reciprocal(out=rs, in_=rs), reads=[b_rs], writes=[b_rs])
                    S.op("dve", lambda e: e.tensor_tensor(out=t1, in0=t1, in1=t2, op=ALU.add), reads=[b_t1, b_t2], writes=[b_t1])

                def s5(rs=rs, b_rs=b_rs, t1=t1, b_t1=b_t1, ob=ob, b_ob=b_ob):
                    S.op("dve", lambda e: e.tensor_tensor(out=ob, in0=t1, in1=rs, op=ALU.mult), reads=[b_t1, b_rs], writes=[b_ob])

                def s6(dst_ap=dst_ap, ob=ob, b_ob=b_ob):
                    S.dma("sp", dst_ap, ob, reads=[b_ob])
                pipe.add([s0, s1, s2, s3, s4, s5, s6])
        pipe.run()
        nb = 0
        for which, cbase in ((0, 5632), (1, 6656)):
            for half in range(2):
                banks = [(nb % 2) * 4 + ts for ts in range(4)]
                mm_tm(c, ring, hT, b_hT, KC, w_v, cbase + half * 256, 256, banks)
                vi = nb % 2
                for ts in range(4):
                    po = psum_f(c, banks[ts], 256)
                    S.op("act", lambda e, po=po, vi=vi, ts=ts: e.copy(out=vb[vi][:, ts, 0:256], in_=po),
                         reads=[c.pbank[banks[ts]]], writes=[b_vb[vi]])
                dst = scr["vtm"][which, t0:t0 + TT, half * 256:(half + 1) * 256].rearrange("(t p) n -> p t n", p=128)
                S.dma("sp", dst, vb[vi], reads=[b_vb[vi]])
                nb += 1
        banks = [(nb % 2) * 4 + ts for ts in range(4)]
        mm_tm(c, ring, hT, b_hT, KC, w_v, 7168, 96, banks)
        for ts in range(4):
            po = psum_f(c, banks[ts], 96)
            S.op("act", lambda e, po=po, ts=ts: e.activation(out=gt[:, ts, :], in_=po, func=AF.Sigmoid),
                 reads=[c.pbank[banks[ts]]], writes=[b_gt])
        S.dma("sp", scr["gates"][t0:t0 + TT, :].rearrange("(t p) n -> p t n", p=128), gt, reads=[b_gt])
        S.barrier(c.dummy)


GELU_C = 1.5957691216057308


def nsa_attn_phase(c, scr, w1, w2, pe, oT_dram, groups=range(4), tgs=range(4)):
    S = c.S
    A = c.arena
    o = c.BASE
    validT = bview(A, o, 1024); o += 1024
    Emat = bview(A, o, 1024); o += 1024
    wmask = bview(A, o, 2048, "p (k n) -> p k n", k=8); o += 2048
    Asel = fview(A, o, 512, "p (t j) -> p t j", j=32); o += 512
    Bsel = fview(A, o, 512, "p (t j) -> p t j", j=32); o += 512
    w1b = [bview(A, o + i * 2048, 2048, "p (j e) -> p j e", j=32) for i in range(2)]; o += 4096
    w2b = [bview(A, o + i * 64, 64) for i in range(2)]; o += 128
    pe_in = bview(A, o, 128, "p (w d) -> p w d", w=2); o += 128
    peT = bview(A, o, 32, "p (w j) -> p w j", w=2); o += 32
    bias = fview(A, o, 2); o += 2
    kcT = bview(A, o, 1024); o += 1024
    vcT = bview(A, o, 1024); o += 1024
    ksT = bview(A, o, 1024); o += 1024
    kwT = bview(A, o, 1024); o += 1024
    vs_aug = bview(A, o, 1040, "p (c n) -> p c n", c=16); o += 1040
    vw_aug = bview(A, o, 1040, "p (c n) -> p c n", c=16); o += 1040
    kcmpT = bview(A, o, 64); o += 64
    rhs_aug = bview(A, o, 81); o += 81
    selT = bview(A, o, 1024); o += 1024
    x1 = fview(A, o, 128); o += 128
    x2 = fview(A, o, 128); o += 128
    hid = bview(A, o, 128, "p (w n) -> p w n", w=2); o += 128
    qT = bview(A, o, 8192, "p (h t) -> p h t", h=8); o += 8192
    slcmask = bview(A, o, 4096, "p (k n) -> p k n", k=16); o += 4096
    eT_s = [bview(A, o + i * 4096, 4096, "p (k n) -> p k n", k=16) for i in range(2)]; o += 8192
    eT_w = [bview(A, o + i * 2048, 2048, "p (k n) -> p k n", k=8) for i in range(2)]; o += 4096
    eT_c = [bview(A, o + i * 256, 256) for i in range(3)]; o += 768
    gates = fview(A, o, 384, "p (t n) -> p t n", t=4); o += 384
    o_comb = [fview(A, o + i * 512, 512, "p (t n) -> p t n", t=4) for i in range(2)]; o += 1024
    coef = fview(A, o, 12); o += 12
    o_b = [bview(A, o + i * 256, 256, "p (t n) -> p t n", t=4) for i in range(2)]; o += 512
    oT_b = [bview(A, o + i * 256, 256) for i in range(2)]; o += 512
    imp_acc = [fview(A, o + i * 128, 128, "p (t j) -> p t j", t=4) for i in range(2)]; o += 256
    impm = fview(A, o, 128, "p (t j) -> p t j", t=4); o += 128
    cmpbuf = fview(A, o, 4096, "p (t j k) -> p t j k", t=4, j=32); o += 4096
    rank = fview(A, o, 128, "p (t j) -> p t j", t=4); o += 128
    sel_b = bview(A, o, 64, "p (t j) -> p t j", t=4); o += 64
    rin = fview(A, o, 12); o += 12
    assert o <= 53000, o

    bc = Buf("attn_const")
    S.dma("pool", validT[0:127, :], c.dr["validT"], writes=[bc])
    S.dma("pool", Emat[0:32, :], c.dr["Emat"], writes=[bc])
    S.dma("pool", wmask, c.dr["wmask"].rearrange("k p n -> p k n"), writes=[bc])
    S.dma("sp", Asel, c.dr["Asel"].rearrange("(t p) j -> p t j", p=128), writes=[bc])
    S.dma("sp", Bsel, c.dr["Bsel"].rearrange("(t p) j -> p t j", p=128), writes=[bc])
    for w in range(2):
        S.dma("pool", w1b[w], w1[w].rearrange("(j d) e -> d j e", d=128), writes=[bc])
        S.dma("pool", w2b[w], w2[w], writes=[bc])
        S.dma("pool", pe_in[0:32, w, :], pe[w], writes=[bc])
    b_rhs = Buf("rhs_aug")
    S.op("dve", lambda e: e.memset(rhs_aug, 0.0), writes=[b_rhs])
    S.op("dve", lambda e: e.memset(rhs_aug[:, 128:129], 1.0), writes=[b_rhs])
    S.dma("pool", rhs_aug[0:127, 129:161], c.dr["ov"], writes=[b_rhs])
    b_vaug = Buf("vaug")
    S.op("dve", lambda e: e.memset(vs_aug[:, :, 128:129], 1.0), writes=[b_vaug])
    S.op("dve", lambda e: e.memset(vw_aug[:, :, 128:129], 1.0), writes=[b_vaug])
    b_peT = Buf("peT")
    for w in range(2):
        pb = psum_b(c, 0)
        S.op("pe", lambda e, w=w, pb=pb: e.transpose(out=pb[:, w * 32:(w + 1) * 32], in_=pe_in[0:32, w, :], identity=c.ident[0:32, 0:32]),
             reads=[bc, c.b_ident], writes=[c.pbank[0]])
    S.op("act", lambda e: e.copy(out=peT, in_=psum_b(c, 0)[:, 0:64].rearrange("p (w j) -> p w j", w=2)), reads=[c.pbank[0]], writes=[b_peT])
    b_bias = Buf("bias")
    for w in range(2):
        po = psum_f(c, 1, 1, w)
        for j in range(32):
            S.op("pe", lambda e, w=w, j=j, po=po: e.matmul(po, lhsT=w1b[w][:, j, :], rhs=peT[:, w, j:j + 1], start=(j == 0), stop=(j == 31)),
                 reads=[bc, b_peT], writes=[c.pbank[1]], sig=(j == 31))
        S.op("act", lambda e, w=w, po=po: e.copy(out=bias[:, w:w + 1], in_=po), reads=[c.pbank[1]], writes=[b_bias])
    S.barrier(c.dummy)

    b_ec = [Buf("eTc%d" % i) for i in range(3)]
    sctr = [0]

    def sbank():
        b = sctr[0] % 4
        sctr[0] += 1
        return b

    for g in groups:
        bg = Buf("grp")
        S.dma("sp", kcT, scr["kT"][0, g], writes=[bg])
        S.dma("sp", vcT, scr["vcT"][g], writes=[bg])
        S.dma("sp", ksT, scr["kT"][1, g], writes=[bg])
        S.dma("sp", kwT, scr["kT"][2, g], writes=[bg])
        S.dma("sp", vs_aug[:, :, 0:128], scr["vtm"][0, :, g * 128:(g + 1) * 128].rearrange("(c p) d -> p c d", p=128), writes=[bg, b_vaug])
        S.dma("sp", vw_aug[:, :, 0:128], scr["vtm"][1, :, g * 128:(g + 1) * 128].rearrange("(c p) d -> p c d", p=128), writes=[bg, b_vaug])
        b_q = Buf("qT")
        S.dma("sp", qT, scr["qT"][g * 8:(g + 1) * 8].rearrange("h d t -> d h t"), writes=[b_q])
        b_x = Buf("x12")
        b_hid = Buf("hid")
        b_kcmp = Buf("kcmp")
        for w, src in ((0, kcT), (1, vcT)):
            src3 = src.rearrange("p (n r) -> p n r", r=16)
            bk = 2 + w
            po = psum_f(c, bk, 127)
            for j in range(32):
                rhs = src3[:, 0:127, j] if j < 16 else src3[:, 1:128, j - 16]
                S.op("pe", lambda e, w=w, j=j, po=po, rhs=rhs: e.matmul(po, lhsT=w1b[w][:, j, :], rhs=rhs, start=(j == 0), stop=(j == 31)),
                     reads=[bc, bg], writes=[c.pbank[bk]], sig=(j == 31))
            S.op("dve", lambda e, w=w, po=po: e.tensor_scalar(out=x1[:, 0:127], in0=po, scalar1=bias[:, w:w + 1], scalar2=None, op0=ALU.add),
                 reads=[c.pbank[bk], b_bias], writes=[b_x])
            S.op("dve", lambda e: e.tensor_tensor(out=x2[:, 0:127], in0=x1[:, 0:127], in1=x1[:, 0:127], op=ALU.mult), reads=[b_x], writes=[b_x])
            S.op("dve", lambda e: e.tensor_scalar(out=x2[:, 0:127], in0=x2[:, 0:127], scalar1=0.044715, scalar2=1.0, op0=ALU.mult, op1=ALU.add),
                 reads=[b_x], writes=[b_x])
            S.op("dve", lambda e: e.tensor_tensor(out=x2[:, 0:127], in0=x2[:, 0:127], in1=x1[:, 0:127], op=ALU.mult), reads=[b_x], writes=[b_x])
            S.op("act", lambda e: e.activation(out=x2[:, 0:127], in_=x2[:, 0:127], func=AF.Sigmoid, scale=GELU_C), reads=[b_x], writes=[b_x])
            S.op("dve", lambda e, w=w: e.tensor_tensor(out=hid[:, w, 0:127], in0=x1[:, 0:127], in1=x2[:, 0:127], op=ALU.mult),
                 reads=[b_x], writes=[b_hid])
            bo = 4 + w
            if w == 0:
                po2 = psum_f(c, bo, 127)
                S.op("pe", lambda e, po2=po2: e.matmul(po2, lhsT=w2b[0], rhs=hid[:, 0, 0:127], start=True, stop=True),
                     reads=[bc, b_hid], writes=[c.pbank[bo]])
                S.op("act", lambda e, po2=po2: e.copy(out=kcmpT[:, 0:127], in_=po2), reads=[c.pbank[bo]], writes=[b_kcmp])
            else:
                po2 = c.psum[0:127, bo * 512: bo * 512 + 128]
                S.op("pe", lambda e, po2=po2: e.matmul(po2, lhsT=hid[:, 1, 0:127], rhs=w2b[1], start=True, stop=True),
                     reads=[bc, b_hid], writes=[c.pbank[bo]])
                S.op("act", lambda e, po2=po2: e.copy(out=rhs_aug[0:127, 0:128], in_=po2), reads=[c.pbank[bo]], writes=[b_rhs])

        b_selT = Buf("selT")
        b_imp = [Buf("imp0"), Buf("imp1")]
        b_impm = Buf("impm")
        b_rin = [Buf("rin%d" % i) for i in range(3)]
        pipe = Pipe()
        idx = 0
        for tg in tgs:
            tsl = slice(tg * 512, (tg + 1) * 512)
            ia, b_ia = imp_acc[tg % 2], b_imp[tg % 2]
            for hl in range(8):
                it_ = idx
                idx += 1
                ec, b_e = eT_c[it_ % 3], b_ec[it_ % 3]
                sb = it_ % 2
                ps = c.psum[0:127, sb * 512:(sb + 1) * 512]
                rb = 2 + it_ % 3
                pr = psum_f(c, rb, 256).rearrange("p (t n) -> p t n", t=4)
                ri, b_ri = rin[:, (it_ % 3) * 4:(it_ % 3) * 4 + 4], b_rin[it_ % 3]

                def s0(ps=ps, sb=sb, hl=hl, tsl=tsl):
                    S.op("pe", lambda e: e.matmul(ps, lhsT=kcmpT[:, 0:127], rhs=qT[:, hl, tsl], start=True, stop=True),
                         reads=[b_kcmp, b_q], writes=[c.pbank[sb]])

                def s1(ps=ps, sb=sb, ec=ec, b_e=b_e):
                    S.op("act", lambda e: e.activation(out=ec[0:127, :], in_=ps, func=AF.Exp), reads=[c.pbank[sb]], writes=[b_e])

                def s2(ec=ec, b_e=b_e, tsl=tsl):
                    S.op("dve", lambda e: e.tensor_tensor(out=ec[0:127, :], in0=ec[0:127, :], in1=validT[0:127, tsl], op=ALU.mult),
                         reads=[b_e, bc], writes=[b_e])

                def s3(ec=ec, b_e=b_e, pr=pr, rb=rb):
                    for qs in range(4):
                        S.op("pe", lambda e, qs=qs: e.matmul(pr[:, qs, 0:33], lhsT=ec[0:127, qs * 128:(qs + 1) * 128],
                                                             rhs=rhs_aug[0:127, 128:161], start=True, stop=True),
                             reads=[b_e, b_rhs], writes=[c.pbank[rb]], sig=(qs == 3))

                def s4(pr=pr, rb=rb, ri=ri, b_ri=b_ri, hl=hl, ia=ia, b_ia=b_ia):
                    if hl == 0:
                        S.op("pool", lambda e: e.memset(ia, 0.0), writes=[b_ia])
                    S.op("dve", lambda e: e.tensor_scalar(out=ri, in0=pr[:, :, 0], scalar1=1e-30, scalar2=None, op0=ALU.max),
                         reads=[c.pbank[rb]], writes=[b_ri])
                    S.op("dve", lambda e: e.reciprocal(out=ri, in_=ri), reads=[b_ri], writes=[b_ri])

                def s5(pr=pr, rb=rb, ri=ri, b_ri=b_ri, ia=ia, b_ia=b_ia):
                    for qs in range(4):
                        S.op("dve", lambda e, qs=qs: e.scalar_tensor_tensor(out=ia[:, qs, :], in0=pr[:, qs, 1:33], scalar=ri[:, qs:qs + 1],
                                                                            in1=ia[:, qs, :], op0=ALU.mult, op1=ALU.add),
                             reads=[c.pbank[rb], b_ri, b_ia], writes=[b_ia])
                pipe.add([s0, s1, s2, s3, s4, s5])
            idx += 1

            def q6(tg=tg, ia=ia, b_ia=b_ia):
                S.op("dve", lambda e: e.tensor_tensor(out=impm, in0=ia, in1=Asel[:, tg * 4:(tg + 1) * 4, :], op=ALU.mult),
                     reads=[b_ia, bc], writes=[b_impm])
                S.op("dve", lambda e: e.tensor_tensor(out=impm, in0=impm, in1=Bsel[:, tg * 4:(tg + 1) * 4, :], op=ALU.add),
                     reads=[b_impm, bc], writes=[b_impm])
                S.op("dve", lambda e: e.tensor_tensor(out=cmpbuf, in0=impm.unsqueeze(2).to_broadcast([128, 4, 32, 32]),
                                                      in1=impm.unsqueeze(3).to_broadcast([128, 4, 32, 32]), op=ALU.is_gt),
                     reads=[b_impm], writes=[b_impm])
                S.op("dve", lambda e: e.tensor_reduce(out=rank, in_=cmpbuf, axis=AX.X, op=ALU.add), reads=[b_impm], writes=[b_impm])
                S.op("dve", lambda e: e.tensor_scalar(out=sel_b, in0=rank, scalar1=15.5, scalar2=None, op0=ALU.is_lt), reads=[b_impm], writes=[b_impm])

            def q7():
                pb = psum_b(c, 5)
                for qs in range(4):
                    S.op("pe", lambda e, qs=qs: e.transpose(out=pb[0:32, qs * 128:(qs + 1) * 128], in_=sel_b[:, qs, :], identity=c.ident),
                         reads=[b_impm, c.b_ident], writes=[c.pbank[5]], sig=(qs == 3))

            def q8(tsl=tsl):
                pb = psum_b(c, 5)
                S.op("act", lambda e: e.copy(out=selT[0:32, tsl], in_=pb[0:32, 0:512]), reads=[c.pbank[5]], writes=[b_selT])
            pipe.add([None] * 6 + [q6, q7, q8])
        pipe.run()

        b_mask = Buf("slcmask")
        b_gates = Buf("gates")
        b_es = [[Buf("eT_s") for _ in range(16)] for _ in range(2)]
        b_ew = [[Buf("eT_w") for _ in range(8)] for _ in range(2)]
        b_oc = [Buf("oc0"), Buf("oc1")]
        b_ob = [Buf("ob0"), Buf("ob1")]
        b_oT = [Buf("oT0"), Buf("oT1")]
        b_coef = [Buf("coef%d" % i) for i in range(3)]
        hi = 0
        for tg in tgs:
            tsl = slice(tg * 512, (tg + 1) * 512)
            S.dma("sp", gates, scr["gates"][tsl, :].rearrange("(t p) n -> p t n", p=128), writes=[b_gates])
            nchunk = 4 * tg + 4
            for ck in range(nchunk):
                sb = sbank()
                ps = psum_f(c, sb)
                S.op("pe", lambda e, ps=ps, ck=ck, tsl=tsl: e.matmul(ps, lhsT=Emat[0:32, ck * 128:(ck + 1) * 128], rhs=selT[0:32, tsl], start=True, stop=True),
                     reads=[bc, b_selT], writes=[c.pbank[sb]])
                if ck < 4 * tg:
                    S.op("act", lambda e, ps=ps, ck=ck: e.copy(out=slcmask[:, ck, :], in_=ps), reads=[c.pbank[sb]], writes=[b_mask])
                else:
                    S.op("dve", lambda e, ps=ps, ck=ck, tg=tg: e.tensor_tensor(out=slcmask[:, ck, :], in0=ps, in1=wmask[:, 4 + ck - 4 * tg, :], op=ALU.mult),
                         reads=[c.pbank[sb], bc], writes=[b_mask])
            pipe = Pipe()
            for hl in range(8):
                h = g * 8 + hl
                i = hi % 2
                hi += 1
                ec, b_e = eT_c[i], b_ec[i]
                es, b_s = eT_s[i], b_es[i]
                ew, b_w = eT_w[i], b_ew[i]
                oc, b_o = o_comb[i], b_oc[i]
                c0 = max(0, 4 * tg - 4)

                def stageA(hl=hl, tg=tg, tsl=tsl, ec=ec, b_e=b_e, es=es, b_s=b_s, ew=ew, b_w=b_w, nchunk=nchunk, c0=c0):
                    th = []
                    t0q = tg * 512

                    def cmp_():
                        sb = sbank()
                        ps = c.psum[0:127, sb * 512:(sb + 1) * 512]
                        S.op("pe", lambda e: e.matmul(ps, lhsT=kcmpT[:, 0:127], rhs=qT[:, hl, tsl], start=True, stop=True),
                             reads=[b_kcmp, b_q], writes=[c.pbank[sb]])
                        S.op("act", lambda e: e.activation(out=ec[0:127, :], in_=ps, func=AF.Exp), reads=[c.pbank[sb]], writes=[b_e])
                        S.op("dve", lambda e: e.tensor_tensor(out=ec[0:127, :], in0=ec[0:127, :], in1=validT[0:127, tsl], op=ALU.mult),
                             reads=[b_e, bc], writes=[b_e])
                    th.append(cmp_)
                    for ck in range(nchunk):
                        lo = max(0, ck - 4 * tg) * 128

                        def slc_(ck=ck, lo=lo):
                            sb = sbank()
                            ps = psum_f(c, sb)
                            S.op("pe", lambda e: e.matmul(ps[:, lo:512], lhsT=ksT[:, ck * 128:(ck + 1) * 128], rhs=qT[:, hl, t0q + lo:t0q + 512], start=True, stop=True),
                                 reads=[bg, b_q], writes=[c.pbank[sb]])
                            S.op("act", lambda e: e.activation(out=es[:, ck, lo:512], in_=ps[:, lo:512], func=AF.Exp), reads=[c.pbank[sb]], writes=[b_s[ck]])
                            S.op("dve", lambda e: e.tensor_tensor(out=es[:, ck, lo:512], in0=es[:, ck, lo:512], in1=slcmask[:, ck, lo:512], op=ALU.mult),
                                 reads=[b_s[ck], b_mask], writes=[b_s[ck]])
                        th.append(slc_)
                    for ck in range(c0, nchunk):
                        wi = 4 + ck - 4 * tg
                        lo, hi_ = max(0, wi - 4) * 128, (min(3, wi) + 1) * 128

                        def win_(ck=ck, wi=wi, lo=lo, hi_=hi_):
                            sb = sbank()
                            ps = psum_f(c, sb)
                            S.op("pe", lambda e: e.matmul(ps[:, lo:hi_], lhsT=kwT[:, ck * 128:(ck + 1) * 128], rhs=qT[:, hl, t0q + lo:t0q + hi_], start=True, stop=True),
                                 reads=[bg, b_q], writes=[c.pbank[sb]])
                            S.op("act", lambda e: e.activation(out=ew[:, ck - c0, lo:hi_], in_=ps[:, lo:hi_], func=AF.Exp), reads=[c.pbank[sb]], writes=[b_w[ck - c0]])
                            S.op("pool", lambda e: e.tensor_tensor(out=ew[:, ck - c0, lo:hi_], in0=ew[:, ck - c0, lo:hi_], in1=wmask[:, wi, lo:hi_], op=ALU.mult),
                                 reads=[b_w[ck - c0], bc], writes=[b_w[ck - c0]])
                        th.append(win_)
                    return th

                def stageB(h=h, tg=tg, ec=ec, b_e=b_e, es=es, b_s=b_s, ew=ew, b_w=b_w, oc=oc, b_o=b_o, c0=c0):
                    th = []

                    def acc_ap(bank0, qs):
                        bk = bank0 + qs // 2
                        return c.psum[:, bk * 512 + (qs % 2) * 256: bk * 512 + (qs % 2) * 256 + 129], bk

                    def epilogue(bank0, br, first):
                        def f():
                            pa = c.psum[:, bank0 * 512:(bank0 + 2) * 512].rearrange("p (t n) -> p t n", t=4)
                            rd = [c.pbank[bank0], c.pbank[bank0 + 1]]
                            cf, b_cf = coef[:, br * 4:br * 4 + 4], b_coef[br]
                            S.op("dve", lambda e: e.tensor_scalar(out=cf, in0=pa[:, :, 128], scalar1=1e-30, scalar2=None, op0=ALU.max), reads=rd, writes=[b_cf])
                            S.op("dve", lambda e: e.reciprocal(out=cf, in_=cf), reads=[b_cf], writes=[b_cf])
                            S.op("dve", lambda e: e.tensor_tensor(out=cf, in0=cf, in1=gates[:, :, br * 32 + h], op=ALU.mult), reads=[b_cf, b_gates], writes=[b_cf])
                            for qs in range(4):
                                if first:
                                    S.op("dve", lambda e, qs=qs: e.tensor_scalar(out=oc[:, qs, :], in0=pa[:, qs, 0:128], scalar1=cf[:, qs:qs + 1], scalar2=None, op0=ALU.mult),
                                         reads=rd + [b_cf], writes=[b_o])
                                else:
                                    S.op("dve", lambda e, qs=qs: e.scalar_tensor_tensor(out=oc[:, qs, :], in0=pa[:, qs, 0:128], scalar=cf[:, qs:qs + 1], in1=oc[:, qs, :],
                                                                                        op0=ALU.mult, op1=ALU.add),
                                         reads=rd + [b_cf, b_o], writes=[b_o])
                        return f
                    for qs in range(4):
                        pa, bk = acc_ap(4, qs)
                        th.append(lambda pa=pa, bk=bk, qs=qs: S.op("pe", lambda e: e.matmul(pa, lhsT=ec[0:127, qs * 128:(qs + 1) * 128], rhs=rhs_aug[0:127, 0:129], start=True, stop=True),
                                                                  reads=[b_e, b_rhs], writes=[c.pbank[bk]]))
                    th.append(epilogue(4, 0, True))
                    for qs in range(4):
                        pa, bk = acc_ap(6, qs)
                        last = 4 * tg + qs
                        for ck in range(last + 1):
                            th.append(lambda pa=pa, bk=bk, qs=qs, ck=ck, last=last: S.op(
                                "pe", lambda e: e.matmul(pa, lhsT=es[:, ck, qs * 128:(qs + 1) * 128], rhs=vs_aug[:, ck, 0:129], start=(ck == 0), stop=(ck == last)),
                                reads=[b_s[ck], bg, b_vaug], writes=[c.pbank[bk]], sig=(ck == last)))
                    th.append(epilogue(6, 1, False))
                    for qs in range(4):
                        pa, bk = acc_ap(4, qs)
                        last = 4 * tg + qs
                        first_ck = max(0, last - 4)
                        for ck in range(first_ck, last + 1):
                            th.append(lambda pa=pa, bk=bk, qs=qs, ck=ck, last=last, first_ck=first_ck: S.op(
                                "pe", lambda e: e.matmul(pa, lhsT=ew[:, ck - c0, qs * 128:(qs + 1) * 128], rhs=vw_aug[:, ck, 0:129], start=(ck == first_ck), stop=(ck == last)),
                                reads=[b_w[ck - c0], bg, b_vaug], writes=[c.pbank[bk]], sig=(ck == last)))
                    th.append(epilogue(4, 2, False))
                    return th

                def stageC(h=h, i=i, tsl=tsl, oc=oc, b_o=b_o):
                    def f():
                        S.op("pool", lambda e: e.tensor_copy(out=o_b[i], in_=oc), reads=[b_o], writes=[b_ob[i]])
                        sb = sbank()
                        pb = psum_b(c, sb)
                        for qs in range(4):
                            S.op("pe", lambda e, qs=qs: e.transpose(out=pb[:, qs * 128:(qs + 1) * 128], in_=o_b[i][:, qs, :], identity=c.ident),
                                 reads=[b_ob[i], c.b_ident], writes=[c.pbank[sb]], sig=(qs == 3))
                        S.op("act", lambda e: e.copy(out=oT_b[i], in_=pb[:, 0:512]), reads=[c.pbank[sb]], writes=[b_oT[i]])
                        S.dma("sp", oT_dram[h * 128:(h + 1) * 128, tsl], oT_b[i], reads=[b_oT[i]])
                    return [f]
                pipe.add([stageA, stageB, stageC])
            pipe.run(interleave=True)
        S.barrier(c.dummy)


def hgrn_proj_phase(c, x_in, gain_row, w_in, lb_logits, scr, ntiles=T // TT):
    S = c.S
    A = c.arena
    o = c.BASE
    hT = bview(A, o, 8192, "p (k n) -> p k n", k=KC); o += 8192
    ring = Ring(c, o); o += 16384
    o_rms = o; o += 16400
    scanm = fview(A, o, 512); o += 512
    lbc = fview(A, o, 64, "p (w h) -> p w h", w=2); o += 64
    lraw = fview(A, o, 512, "p (w d) -> p w d", w=4); o += 512
    ident_f = fview(A, o, 128); o += 128
    decay = fview(A, o, 1024, "p (h n) -> p h n", h=32); o += 1024
    vb = [bview(A, o + i * 512, 512, "p (t n) -> p t n", t=4) for i in range(2)]; o += 1024
    b_vb = [Buf("vb0"), Buf("vb1")]
    assert o <= 53000, o
    oo = o_rms
    R = {}
    for nm, words, bf, depth in (("a", 512, False, 3), ("f", 512, False, 2), ("lg", 512, False, 2), ("gc", 512, False, 2),
                                 ("kk", 512, False, 5), ("eg", 512, False, 2), ("en", 512, False, 2), ("gt", 512, False, 2),
                                 ("et", 512, False, 2), ("qd", 256, True, 3), ("kinv", 256, True, 3), ("ktT", 256, True, 3),
                                 ("ktl", 256, True, 3)):
        views = []
        for i in range(depth):
            views.append(bview(A, oo, words) if bf else fview(A, oo, words))
            oo += words
        R[nm] = Rot(views)
    assert oo <= o_rms + 16400, oo

    bc = Buf("hconst")
    S.dma("sp", scanm, c.dr["scanm"], writes=[bc])
    S.dma("sp", ident_f, c.dr["ident"], writes=[bc])
    S.dma("sp", lraw[0:32, 0, :], lb_logits[0].rearrange("(h d) -> h d", d=128), writes=[bc])
    S.dma("sp", lraw[0:32, 1, :], lb_logits[1].rearrange("(h d) -> h d", d=128), writes=[bc])
    S.op("act", lambda e: e.activation(out=lraw[0:32, 0:2, :], in_=lraw[0:32, 0:2, :], func=AF.Exp), reads=[bc], writes=[bc])
    S.op("dve", lambda e: e.tensor_tensor(out=lraw[0:32, 2, :], in0=lraw[0:32, 0, :], in1=lraw[0:32, 1, :], op=ALU.add), reads=[bc], writes=[bc])
    S.op("dve", lambda e: e.reciprocal(out=lraw[0:32, 2, :], in_=lraw[0:32, 2, :]), reads=[bc], writes=[bc])
    S.op("dve", lambda e: e.tensor_tensor(out=lraw[0:32, 3, :], in0=lraw[0:32, 1, :], in1=lraw[0:32, 2, :], op=ALU.mult), reads=[bc], writes=[bc])
    S.op("dve", lambda e: e.tensor_tensor(out=lraw[0:32, 2, :], in0=lraw[0:32, 0, :], in1=lraw[0:32, 2, :], op=ALU.mult), reads=[bc], writes=[bc])
    for w, srcw in ((0, 3), (1, 2)):
        po = psum_f(c, 0, 32, w * 32)
        S.op("pe", lambda e, po=po, srcw=srcw: e.transpose(out=po, in_=lraw[0:32, srcw, :], identity=ident_f[0:32, 0:32]),
             reads=[bc], writes=[c.pbank[0]])
    S.op("act", lambda e: e.copy(out=lbc, in_=psum_f(c, 0, 64).rearrange("p (w h) -> p w h", w=2)), reads=[c.pbank[0]], writes=[bc])
    b_dec = Buf("decay")

    w_v = w_in.rearrange("(k p) n -> p k n", p=128)
    for tt in range(ntiles):
        t0 = tt * TT
        b_hT = Buf("hT")
        rms_to_hT(c, x_in, gain_row, hT, b_hT, o_rms, t0)
        S.barrier(c.dummy)
        pipe = Pipe()
        wf, wq = {}, {}
        for h in range(32):
            hp, cc = h // 2, h % 2
            fb = h % 2
            qb = 2 + h % 3
            tb = 5 + h % 2
            pf, pq = psum_f(c, fb), psum_f(c, qb)
            pb = psum_b(c, tb)
            a, b_a = R["a"].get(h)
            f, b_f = R["f"].get(h)
            lg, b_lg = R["lg"].get(h)
            gc, b_gc = R["gc"].get(h)
            kk, b_kk = R["kk"].get(h)
            eg, b_eg = R["eg"].get(h)
            en, b_en = R["en"].get(h)
            gt_, b_gt = R["gt"].get(h)
            et, b_et = R["et"].get(h)
            qd, b_qd = R["qd"].get(h)
            kinv, b_kinv = R["kinv"].get(h)
            ktT, b_ktT = R["ktT"].get(h)
            ktl_, b_ktl = R["ktl"].get(h)
            ktl = ktl_.rearrange("p (t n) -> p t n", t=4)
            gc3 = gc.rearrange("p (n r) -> p n r", r=64)

            def mm(wd, col0, po, bank, hp=hp, cc=cc):
                if cc == 0:
                    wd[hp] = ring.load(c, w_v[:, 0:KC, col0:col0 + 256], KC, 256)
                wt, wb = wd[hp]
                for kc in range(KC):
                    S.op("pe", lambda e, kc=kc: e.matmul(po, lhsT=wt[:, kc, cc * 128:(cc + 1) * 128], rhs=hT[:, kc, :],
                                                         start=(kc == 0), stop=(kc == KC - 1)),
                         reads=[wb, b_hT], writes=[c.pbank[bank]], sig=(kc == KC - 1))

            def s0(mm=mm, hp=hp, pf=pf, fb=fb):
                mm(wf, 4096 + hp * 256, pf, fb)

            def s1(a=a, b_a=b_a, pf=pf, fb=fb):
                S.op("act", lambda e: e.activation(out=a, in_=pf, func=AF.Exp, scale=-1.0), reads=[c.pbank[fb]], writes=[b_a])

            def s2(a=a, b_a=b_a):
                S.op("dve", lambda e: e.tensor_scalar(out=a, in0=a, scalar1=1.0, scalar2=None, op0=ALU.add), reads=[b_a], writes=[b_a])

            def s3(a=a, b_a=b_a, f=f, b_f=b_f, h=h):
                S.op("dve", lambda e: e.reciprocal(out=a, in_=a), reads=[b_a], writes=[b_a])
                S.op("dve", lambda e: e.tensor_scalar(out=f, in0=a, scalar1=lbc[:, 1, h:h + 1], scalar2=lbc[:, 0, h:h + 1],
                                                      op0=ALU.mult, op1=ALU.add), reads=[b_a, bc], writes=[b_f])

            def s4(f=f, b_f=b_f, lg=lg, b_lg=b_lg, kk=kk, b_kk=b_kk):
                S.op("act", lambda e: e.activation(out=lg, in_=f, func=AF.Ln), reads=[b_f], writes=[b_lg])
                S.op("dve", lambda e: e.tensor_scalar(out=kk, in0=f, scalar1=-1.0, scalar2=1.0, op0=ALU.mult, op1=ALU.add),
                     reads=[b_f], writes=[b_kk])

            def s5(mm=mm, hp=hp, pq=pq, qb=qb, lg=lg, b_lg=b_lg, gc=gc, b_gc=b_gc):
                S.op("dve", lambda e: e.tensor_tensor_scan(out=gc, data0=scanm, data1=lg, initial=0.0, op0=ALU.mult, op1=ALU.add),
                     reads=[b_lg, bc], writes=[b_gc])
                mm(wq, hp * 256, pq, qb)

            def s6(gc=gc, gc3=gc3, b_gc=b_gc, eg=eg, b_eg=b_eg, en=en, b_en=b_en, gt_=gt_, b_gt=b_gt, h=h, tt=tt):
                S.op("act", lambda e: e.activation(out=eg, in_=gc, func=AF.Exp), reads=[b_gc], writes=[b_eg])
                S.op("act", lambda e: e.activation(out=en, in_=gc, func=AF.Exp, scale=-1.0), reads=[b_gc], writes=[b_en])
                S.op("act", lambda e: e.activation(out=decay[:, h, tt * 8:(tt + 1) * 8], in_=gc3[:, :, 63], func=AF.Exp),
                     reads=[b_gc], writes=[b_dec])
                S.op("dve", lambda e: e.tensor_tensor(out=gt_.rearrange("p (n r) -> p n r", r=64), in0=gc3[:, :, 63:64].to_broadcast([128, 8, 64]),
                                                      in1=gc3, op=ALU.subtract), reads=[b_gc], writes=[b_gt])

            def s7(pq=pq, qb=qb, eg=eg, b_eg=b_eg, qd=qd, b_qd=b_qd, kk=kk, b_kk=b_kk, en=en, b_en=b_en, kinv=kinv, b_kinv=b_kinv,
                   gt_=gt_, b_gt=b_gt, et=et, b_et=b_et):
                S.op("dve", lambda e: e.tensor_tensor(out=qd, in0=pq, in1=eg, op=ALU.mult), reads=[c.pbank[qb], b_eg], writes=[b_qd])
                S.op("dve", lambda e: e.tensor_tensor(out=kinv, in0=kk, in1=en, op=ALU.mult), reads=[b_kk, b_en], writes=[b_kinv])
                S.op("act", lambda e: e.activation(out=et, in_=gt_, func=AF.Exp), reads=[b_gt], writes=[b_et])

            def s8(kk=kk, b_kk=b_kk, et=et, b_et=b_et, ktT=ktT, b_ktT=b_ktT, qd=qd, b_qd=b_qd, kinv=kinv, b_kinv=b_kinv, h=h, t0=t0):
                S.op("dve", lambda e: e.tensor_tensor(out=ktT, in0=kk, in1=et, op=ALU.mult), reads=[b_kk, b_et], writes=[b_ktT])
                S.dma("sp", scr["qdT"][h][:, t0:t0 + TT], qd, reads=[b_qd])
                S.dma("sp", scr["kinvT"][h][:, t0:t0 + TT], kinv, reads=[b_kinv])

            def s9(pb=pb, tb=tb, ktT=ktT, b_ktT=b_ktT):
                for ts in range(4):
                    S.op("pe", lambda e, ts=ts: e.transpose(out=pb[:, ts * 128:(ts + 1) * 128], in_=ktT[:, ts * 128:(ts + 1) * 128], identity=c.ident),
                         reads=[b_ktT, c.b_ident], writes=[c.pbank[tb]], sig=(ts == 3))

            def s10(pb=pb, tb=tb, ktl=ktl, b_ktl=b_ktl):
                S.op("act", lambda e: e.copy(out=ktl, in_=pb[:, 0:512].rearrange("p (t n) -> p t n", t=4)), reads=[c.pbank[tb]], writes=[b_ktl])

            def s11(ktl=ktl, b_ktl=b_ktl, h=h, t0=t0):
                S.dma("sp", scr["ktail"][t0:t0 + TT, h * 128:(h + 1) * 128].rearrange("(t p) d -> p t d", p=128), ktl, reads=[b_ktl])
            pipe.add([s0, s1, s2, s3, s4, s5, s6, s7, s8, s9, s10, s11])
        pipe.run()
        nb = 0
        for which, cbase, dst in ((0, 8192, scr["v"]), (1, 12288, scr["sgz"])):
            for blk in range(16):
                banks = [(nb % 2) * 4 + ts for ts in range(4)]
                mm_tm(c, ring, hT, b_hT, KC, w_v, cbase + blk * 256, 256, banks)
                vi = nb % 2
                for ts in range(4):
                    po = psum_f(c, banks[ts], 256)
                    if which == 0:
                        S.op("act", lambda e, po=po, vi=vi, ts=ts: e.copy(out=vb[vi][:, ts, :], in_=po), reads=[c.pbank[banks[ts]]], writes=[b_vb[vi]])
                    else:
                        S.op("act", lambda e, po=po, vi=vi, ts=ts: e.activation(out=vb[vi][:, ts, :], in_=po, func=AF.Silu), reads=[c.pbank[banks[ts]]], writes=[b_vb[vi]])
                S.dma("sp", dst[t0:t0 + TT, blk * 256:(blk + 1) * 256].rearrange("(t p) n -> p t n", p=128), vb[vi], reads=[b_vb[vi]])
                nb += 1
        S.barrier(c.dummy)
    S.dma("sp", scr["decay"], decay.rearrange("p h n -> p (h n)"), reads=[b_dec])
    S.barrier(c.dummy)


def hgrn_rec_phase(c, scr, o_norm, oT_dram, nchunks=32):
    S = c.S
    A = c.arena
    o = c.BASE
    qd_t = bview(A, o, 8192, "p (h t) -> p h t", h=32); o += 8192
    ki_t = bview(A, o, 8192, "p (h t) -> p h t", h=32); o += 8192
    tok = []
    for i in range(2):
        d = {}
        for k in ("kt", "v", "sg"):
            d[k] = bview(A, o, 2048); o += 2048
        d["b"] = Buf("tok%d" % i)
        tok.append(d)
    S_f = fview(A, o, 4096, "p (h n) -> p h n", h=32); o += 4096
    S_b = bview(A, o, 2048, "p (h n) -> p h n", h=32); o += 2048
    decay = fview(A, o, 1024, "p (h n) -> p h n", h=32); o += 1024
    o_all = [fview(A, o + i * 4096, 4096, "p (h n) -> p h n", h=32) for i in range(2)]; o += 8192
    yb = bview(A, o, 2048); o += 2048
    aTm = [bview(A, o + i * 128, 128, "p (h n) -> p h n", h=4) for i in range(2)]; o += 256
    oT_c = [bview(A, o + i * 1024, 1024, "p (k n) -> p k n", k=32) for i in range(2)]; o += 2048
    m64 = bview(A, o, 32); o += 32
    gn = fview(A, o, 128); o += 128
    ssr = fview(A, o, 96, "p (w h) -> p w h", w=3); o += 96
    assert o <= 53000, o

    bc = Buf("rconst")
    S.dma("pool", m64[0:64, :], c.dr["m64"], writes=[bc])
    S.dma("sp", gn[0:64, :], o_norm.partition_broadcast(64), writes=[bc])
    S.dma("sp", decay, scr["decay"].rearrange("p (h n) -> p h n", h=32), writes=[bc])
    b_S = [Buf("S%d" % i) for i in range(8)]
    for i in range(8):
        S.op("pool", lambda e, i=i: e.memset(S_f[:, i * 4:(i + 1) * 4, :], 0.0), writes=[b_S[i]])
        S.op("pool", lambda e, i=i: e.memset(S_b[:, i * 4:(i + 1) * 4, :], 0.0), writes=[b_S[i]])
    b_fm = Buf("fm_tiles")
    b_oall = [Buf("o_all0"), Buf("o_all1")]
    b_yb = Buf("yb")
    b_ss = Buf("ssr")
    b_aT = [Buf("aTm0"), Buf("aTm1")]
    b_oT = [Buf("oTc0"), Buf("oTc1")]
    pipe = Pipe()
    it = 0
    for n in range(nchunks):
        tt, cl = n // 8, n % 8
        csl = slice(cl * 64, (cl + 1) * 64)
        tk = tok[n % 2]
        bt = tk["b"]
        oa, b_oa = o_all[n % 2], b_oall[n % 2]
        for hg in range(8):
            idx = it
            it += 1
            ai = idx % 2
            ba, bo, bd = idx % 2, 2 + idx % 2, 4 + idx % 3
            pa = c.psum[0:64, ba * 512: ba * 512 + 256].rearrange("p (h n) -> p h n", h=4)
            po = c.psum[0:64, bo * 512:(bo + 1) * 512].rearrange("p (h n) -> p h n", h=4)
            pd = psum_f(c, bd).rearrange("p (h n) -> p h n", h=4)
            hsl = slice(hg * 4, (hg + 1) * 4)

            def s0(n=n, tt=tt, cl=cl, hg=hg, tk=tk, bt=bt, pa=pa, ba=ba, csl=csl):
                if hg == 0:
                    if cl == 0:
                        S.dma("sp", qd_t, scr["qdT"][:, :, tt * 512:(tt + 1) * 512].rearrange("h d t -> d h t"), writes=[b_fm])
                        S.dma("sp", ki_t, scr["kinvT"][:, :, tt * 512:(tt + 1) * 512].rearrange("h d t -> d h t"), writes=[b_fm])
                    S.dma("sp", tk["kt"][0:64, :], scr["ktail"][n * 64:(n + 1) * 64, :], writes=[bt])
                    S.dma("sp", tk["v"][0:64, :], scr["v"][n * 64:(n + 1) * 64, :], writes=[bt])
                    S.dma("sp", tk["sg"][0:64, :], scr["sgz"][n * 64:(n + 1) * 64, :], writes=[bt])
                for j in range(4):
                    h = hg * 4 + j
                    S.op("pe", lambda e, j=j, h=h: e.matmul(pa[:, j, :], lhsT=ki_t[:, h, csl], rhs=qd_t[:, h, csl], start=True, stop=True),
                         reads=[b_fm], writes=[c.pbank[ba]], sig=(j == 3))

            def s1(pa=pa, ba=ba, ai=ai):
                S.op("dve", lambda e: e.tensor_tensor(out=aTm[ai][0:64, :, :], in0=pa, in1=m64[0:64, :].unsqueeze(1).to_broadcast([64, 4, 64]), op=ALU.mult),
                     reads=[c.pbank[ba], bc], writes=[b_aT[ai]])

            def s2(hg=hg, tk=tk, bt=bt, po=po, bo=bo, pd=pd, bd=bd, ai=ai, csl=csl):
                for j in range(4):
                    h = hg * 4 + j
                    hs = slice(h * 128, (h + 1) * 128)
                    S.op("pe", lambda e, j=j, hs=hs: e.matmul(po[:, j, :], lhsT=aTm[ai][0:64, j, :], rhs=tk["v"][0:64, hs], start=True, stop=False),
                         reads=[b_aT[ai], bt], writes=[c.pbank[bo]], sig=False)
                    S.op("pe", lambda e, j=j, h=h: e.matmul(po[:, j, :], lhsT=qd_t[:, h, csl], rhs=S_b[:, h, :], start=False, stop=True),
                         reads=[b_fm, b_S[hg]], writes=[c.pbank[bo]], sig=(j == 3))
                for j in range(4):
                    h = hg * 4 + j
                    hs = slice(h * 128, (h + 1) * 128)
                    S.op("pe", lambda e, j=j, hs=hs: e.matmul(pd[:, j, :], lhsT=tk["kt"][0:64, hs], rhs=tk["v"][0:64, hs], start=True, stop=True),
                         reads=[bt], writes=[c.pbank[bd]], sig=(j == 3))

            def s3(hg=hg, hsl=hsl, po=po, bo=bo, oa=oa, b_oa=b_oa, n=n):
                S.op("act", lambda e: e.copy(out=oa[0:64, hsl, :], in_=po), reads=[c.pbank[bo]], writes=[b_oa])
                S.op("dve", lambda e: e.tensor_tensor(out=S_f[:, hsl, :], in0=S_f[:, hsl, :], in1=decay[:, hsl, n:n + 1].to_broadcast([128, 4, 128]), op=ALU.mult),
                     reads=[b_S[hg], bc], writes=[b_S[hg]])

            def s4(hg=hg, hsl=hsl, pd=pd, bd=bd):
                S.op("dve", lambda e: e.tensor_tensor(out=S_f[:, hsl, :], in0=S_f[:, hsl, :], in1=pd, op=ALU.add),
                     reads=[b_S[hg], c.pbank[bd]], writes=[b_S[hg]])

            def s5(hg=hg, hsl=hsl):
                S.op("act", lambda e: e.copy(out=S_b[:, hsl, :], in_=S_f[:, hsl, :]), reads=[b_S[hg]], writes=[b_S[hg]])
            pipe.add([s0, s1, s2, s3, s4, s5])
        it += 1
        oi = n % 2
        yb3 = yb.rearrange("p (h n) -> p h n", h=32)

        def t5(oa=oa, b_oa=b_oa):
            S.op("act", lambda e: e.activation(out=yb3[0:64], in_=oa[0:64], func=AF.Square), reads=[b_oa], writes=[b_yb])

        def t6():
            S.op("dve", lambda e: e.tensor_reduce(out=ssr[0:64, 0, :], in_=yb3[0:64], axis=AX.X, op=ALU.add), reads=[b_yb], writes=[b_ss])
            S.op("dve", lambda e: e.tensor_scalar(out=ssr[0:64, 1, :], in0=ssr[0:64, 0, :], scalar1=1.0 / 128, scalar2=EPS, op0=ALU.mult, op1=ALU.add),
                 reads=[b_ss], writes=[b_ss])

        def t7():
            S.op("act", lambda e: e.activation(out=ssr[0:64, 1, :], in_=ssr[0:64, 1, :], func=AF.Sqrt), reads=[b_ss], writes=[b_ss])

        def t8(oa=oa, b_oa=b_oa):
            S.op("dve", lambda e: e.reciprocal(out=ssr[0:64, 2, :], in_=ssr[0:64, 1, :]), reads=[b_ss], writes=[b_ss])
            S.op("dve", lambda e: e.tensor_tensor(out=oa[0:64], in0=oa[0:64], in1=ssr[0:64, 2, :].unsqueeze(2).to_broadcast([64, 32, 128]), op=ALU.mult),
                 reads=[b_oa, b_ss], writes=[b_oa])

        def t9(oa=oa, b_oa=b_oa):
            S.op("pool", lambda e: e.tensor_tensor(out=oa[0:64], in0=oa[0:64], in1=gn[0:64, :].unsqueeze(1).to_broadcast([64, 32, 128]), op=ALU.mult),
                 reads=[b_oa, bc], writes=[b_oa])

        def t10(oa=oa, b_oa=b_oa, tk=tk, bt=bt):
            S.op("dve", lambda e: e.tensor_tensor(out=yb[0:64, :], in0=oa[0:64].rearrange("p h n -> p (h n)"), in1=tk["sg"][0:64, :], op=ALU.mult),
                 reads=[b_oa, bt, b_yb], writes=[b_yb])

        def tT(half):
            def f():
                pb = psum_b(c, 7).rearrange("p (k n) -> p k n", k=16)
                for j in range(16):
                    kc = half * 16 + j
                    S.op("pe", lambda e, j=j, kc=kc: e.transpose(out=pb[:, j, :], in_=yb[0:64, kc * 128:(kc + 1) * 128], identity=c.ident[0:64, 0:64]),
                         reads=[b_yb, c.b_ident], writes=[c.pbank[7]], sig=(j == 15))
            return f

        def tC(half, oi=oi):
            def f():
                pb = psum_b(c, 7).rearrange("p (k n) -> p k n", k=16)
                S.op("act", lambda e: e.copy(out=oT_c[oi][:, half * 16:(half + 1) * 16, :], in_=pb), reads=[c.pbank[7]], writes=[b_oT[oi]])
            return f

        def t15(n=n, oi=oi):
            S.dma("sp", oT_dram[:, n * 64:(n + 1) * 64].rearrange("(k p) t -> p k t", p=128), oT_c[oi], reads=[b_oT[oi]])
        pipe.add([None] * 5 + [t5, t6, t7, t8, t9, t10, tT(0), tC(0), tT(1), tC(1), t15])
    pipe.run()
    S.barrier(c.dummy)


def make_consts():
    cs = {}
    cs["ident"] = np.eye(128, dtype=np.float32)
    cs["ones"] = np.ones((128, 128), np.float32)
    P = np.zeros((128, 128), np.float32)
    for dp in range(64):
        P[dp + 64, dp] = -1.0
    for dp in range(64, 128):
        P[dp - 64, dp] = 1.0
    cs["prot"] = P
    inv = (np.float32(10000.0) ** (-np.arange(0, 128, 2, dtype=np.float32) / np.float32(128))).astype(np.float32)
    ang = (np.arange(T, dtype=np.float32)[:, None] * inv[None, :]).astype(np.float32)
    ang = np.concatenate([ang, ang], axis=-1)
    cs["cosT"] = np.ascontiguousarray(np.cos(ang).astype(np.float32).T)
    cs["sinT"] = np.ascontiguousarray(np.sin(ang).astype(np.float32).T)
    n = np.arange(127)
    t = np.arange(T)
    cs["validT"] = ((16 * n[:, None] + 31) <= t[None, :]).astype(np.float32)
    j = np.arange(32)
    cs["ov"] = ((16 * n[:, None] < 64 * j[None, :] + 64) & (16 * n[:, None] + 32 > 64 * j[None, :])).astype(np.float32)
    cs["Emat"] = ((t[None, :] // 64) == j[:, None]).astype(np.float32)
    kl = np.arange(128)[:, None]
    tl = np.arange(512)[None, :]
    wm = np.zeros((8, 128, 512), np.float32)
    for i in range(8):
        dlt = (i - 4) * 128
        df = tl - kl - dlt
        wm[i] = ((df >= 0) & (df < 512)).astype(np.float32)
    cs["wmask"] = wm
    cur = t // 64
    forced = (j[None, :] == 0) | (j[None, :] == cur[:, None]) | (j[None, :] == cur[:, None] - 1)
    causal = (j[None, :] * 64) <= t[:, None]
    cs["Asel"] = (causal & ~forced).astype(np.float32)
    cs["Bsel"] = np.where(forced, 1e30, np.where(causal, 0.0, -1e30)).astype(np.float32)
    cs["scanm"] = np.tile((np.arange(512) % 64 != 0).astype(np.float32)[None, :], (128, 1))
    cs["m64"] = (np.arange(64)[:, None] <= np.arange(64)[None, :]).astype(np.float32)
    return cs


HG_IN = 16384
_CACHE = {}


def build_program():
    nc = bass.Bass("TRN2", target_bir_lowering=False)
    dr = {}

    def inp(name, shape):
        dr[name] = nc.dram_tensor(name, list(shape), F32, kind="ExternalInput").ap()

    inp("x", [T, D])
    inp("ffn_norm", [4, D])
    inp("ffn_w_gate", [4, D, F])
    inp("ffn_w_up", [4, D, F])
    inp("ffn_w_down", [4, F, D])
    inp("mix_norm", [2, D])
    inp("nsa_w_in", [D, NSA_IN])
    inp("nsa_q_norm", [1, 128])
    inp("nsa_k_norm", [1, 3, 128])
    inp("nsa_cmp_pos", [2, 32, 128])
    inp("nsa_cmp_w1", [2, 4096, 128])
    inp("nsa_cmp_w2", [2, 128, 128])
    inp("nsa_w_o", [D, D])
    inp("hgrn_w_in", [D, HG_IN])
    inp("hgrn_lb_logits", [2, D])
    inp("hgrn_o_norm", [1, 128])
    inp("hgrn_w_o", [D, D])
    cs = make_consts()
    for k, v in cs.items():
        inp(k, v.shape)
    out = nc.dram_tensor("out", [T, D], F32, kind="ExternalOutput").ap()
    xa = nc.dram_tensor("s_xa", [T, D], F32).ap()
    xb = nc.dram_tensor("s_xb", [T, D], F32).ap()
    nscr = {
        "qT": nc.dram_tensor("s_qT", [32, 128, T], BF16).ap(),
        "kT": nc.dram_tensor("s_kT", [3, 4, 128, T], BF16).ap(),
        "vcT": nc.dram_tensor("s_vcT", [4, 128, T], BF16).ap(),
        "vtm": nc.dram_tensor("s_vtm", [2, T, 512], BF16).ap(),
        "gates": nc.dram_tensor("s_gates", [T, 96], F32).ap(),
    }
    hscr = {
        "qdT": nc.dram_tensor("s_qdT", [32, 128, T], BF16).ap(),
        "kinvT": nc.dram_tensor("s_kinvT", [32, 128, T], BF16).ap(),
        "ktail": nc.dram_tensor("s_ktail", [T, D], BF16).ap(),
        "v": nc.dram_tensor("s_v", [T, D], BF16).ap(),
        "sgz": nc.dram_tensor("s_sgz", [T, D], BF16).ap(),
        "decay": nc.dram_tensor("s_decay", [128, 1024], F32).ap(),
    }
    oT = nc.dram_tensor("s_oT", [D, T], BF16).ap()
    with ExitStack() as st:
        c = Ctx()
        c.nc = nc
        c.dr = dr
        c.arena = st.enter_context(nc.sbuf_tensor("arena", [128, 53000], F32))
        c.psum = st.enter_context(nc.psum_tensor("ps", [128, 4096], F32))
        c.S = Sched(nc, st)
        setup_common(c)

        def ffn(i, xi, xo):
            ffn_phase(c, xi, xo, dr["ffn_norm"][i:i + 1, :], dr["ffn_w_gate"][i], dr["ffn_w_up"][i], dr["ffn_w_down"][i])

        ffn(0, dr["x"], xa)
        nsa_proj_phase(c, xa, dr["mix_norm"][0:1, :], dr["nsa_w_in"], dr["nsa_q_norm"], dr["nsa_k_norm"], nscr)
        nsa_attn_phase(c, nscr, dr["nsa_cmp_w1"], dr["nsa_cmp_w2"], dr["nsa_cmp_pos"], oT)
        out_proj_phase(c, oT, dr["nsa_w_o"], xa, xb, 1.0)
        ffn(1, xb, xa)
        ffn(2, xa, xb)
        hgrn_proj_phase(c, xb, dr["mix_norm"][1:2, :], dr["hgrn_w_in"], dr["hgrn_lb_logits"], hscr)
        hgrn_rec_phase(c, hscr, dr["hgrn_o_norm"], oT)
        out_proj_phase(c, oT, dr["hgrn_w_o"], xb, xa, 1.0)
        ffn(3, xa, out)
        c.S.final_wait("sp")
        c.S.emit()
    return nc, cs


def kernel(x, ffn_norm, ffn_w_gate, ffn_w_up, ffn_w_down, mix_norm, nsa_w_in, nsa_q_norm, nsa_k_norm,
           nsa_cmp_pos, nsa_cmp_w1, nsa_cmp_w2, nsa_w_o, hgrn_w_in, hgrn_lb_logits, hgrn_o_norm, hgrn_w_o):
    if "nc" not in _CACHE:
        _CACHE["nc"] = build_program()
    nc, cs = _CACHE["nc"]
    f32 = lambda a: np.ascontiguousarray(np.asarray(a), dtype=np.float32)
    shared = {
        "ffn_norm": f32(ffn_norm).reshape(4, D),
        "ffn_w_gate": f32(ffn_w_gate).reshape(4, D, F),
        "ffn_w_up": f32(ffn_w_up).reshape(4, D, F),
        "ffn_w_down": f32(ffn_w_down).reshape(4, F, D),
        "mix_norm": f32(mix_norm).reshape(2, D),
        "nsa_w_in": f32(nsa_w_in).reshape(D, NSA_IN),
        "nsa_q_norm": f32(nsa_q_norm).reshape(1, 128),
        "nsa_k_norm": f32(nsa_k_norm).reshape(1, 3, 128),
        "nsa_cmp_pos": f32(nsa_cmp_pos).reshape(2, 32, 128),
        "nsa_cmp_w1": f32(nsa_cmp_w1).reshape(2, 4096, 128),
        "nsa_cmp_w2": f32(nsa_cmp_w2).reshape(2, 128, 128),
        "nsa_w_o": f32(nsa_w_o).reshape(D, D),
        "hgrn_w_in": f32(hgrn_w_in).reshape(D, HG_IN),
        "hgrn_lb_logits": f32(hgrn_lb_logits).reshape(2, D),
        "hgrn_o_norm": f32(hgrn_o_norm).reshape(1, 128),
        "hgrn_w_o": f32(hgrn_w_o).reshape(D, D),
    }
    shared.update(cs)
    xs = f32(x)
    n = xs.shape[0]
    in_maps = []
    for i in range(n):
        m = dict(shared)
        m["x"] = xs[i]
        in_maps.append(m)
    res = run_bass_kernel_spmd(nc, in_maps, core_ids=list(range(n)))
    return np.stack([np.asarray(r["out"], dtype=np.float32) for r in res.results], axis=0)
```
